# Optimizing a Trainium2 kernel written in Bass

```python
import math
import jax, jax.numpy as jnp
from jax import lax
import numpy as np

D_MODEL = 2048
BATCH = 1
SEQ = 8192
DEPTH = 1

D_MIX = D_MODEL
RET_HEADS = 4
RET_HEAD_DIM = D_MIX // 2 // RET_HEADS
RET_WIDTH = RET_HEADS * RET_HEAD_DIM
RET_CHUNK = 128
ROPE_THETA = 10000.0
SSM_WIDTH = D_MIX - RET_WIDTH
SSM_HEAD_DIM = 64
SSM_HEADS = SSM_WIDTH // SSM_HEAD_DIM
SSM_GROUPS = 2
SSM_STATE = 128
SSM_CONV = 4
SSM_CHUNK = 128
SSM_CONV_DIM = SSM_WIDTH + 2 * SSM_GROUPS * SSM_STATE
IN_PROJ_DIM = 4 * RET_WIDTH + SSM_WIDTH + SSM_CONV_DIM + SSM_HEADS
N_EXPERT_GROUPS = 4
EXPERTS_PER_GROUP = 8
N_EXPERTS = N_EXPERT_GROUPS * EXPERTS_PER_GROUP
TOP_K = 2
D_EXPERT = D_MODEL // 4
EPS = 1e-6

kernel_name = 'hymba_retention_ssd_hmoe_block'


def rmsnorm(x, w):
    xf = x.astype(jnp.float32)
    y = xf * lax.rsqrt(jnp.mean(xf * xf, axis=-1, keepdims=True) + EPS)
    return (y * w.astype(jnp.float32)).astype(x.dtype)


def rope(x, positions):
    half = x.shape[-1] // 2
    inv = ROPE_THETA ** (-jnp.arange(half, dtype=jnp.float32) / half)
    ang = positions.astype(jnp.float32)[..., None] * inv
    cos = jnp.cos(ang)[:, :, None, :]
    sin = jnp.sin(ang)[:, :, None, :]
    x1, x2 = x[..., :half], x[..., half:]
    return jnp.concatenate([x1 * cos - x2 * sin, x2 * cos + x1 * sin], axis=-1)


def retention(q, k, v, positions):
    Bsz, T, H, Dh = q.shape
    Lc = RET_CHUNK
    C = T // Lc
    q = rope(q, positions)
    k = rope(k, positions) * (Dh ** -0.5)
    log_gamma = jnp.log1p(-(2.0 ** (-5.0 - jnp.arange(H, dtype=jnp.float32))))
    q = q.reshape(Bsz, C, Lc, H, Dh)
    k = k.reshape(Bsz, C, Lc, H, Dh)
    v = v.reshape(Bsz, C, Lc, H, Dh)
    idx = jnp.arange(Lc, dtype=jnp.float32)
    diff = idx[:, None] - idx[None, :]
    causal = diff >= 0
    decay_intra = jnp.where(causal[None], jnp.exp(jnp.where(causal, diff, 0.0)[None] * log_gamma[:, None, None]), 0.0)
    scores = jnp.einsum('bclhd,bcshd->bchls', q, k) * decay_intra[None, None]
    y_intra = jnp.einsum('bchls,bcshd->bclhd', scores, v)
    w_state = jnp.exp((Lc - 1.0 - idx)[None, :] * log_gamma[:, None])
    v_w = v * w_state.T[None, None, :, :, None]
    chunk_states = jnp.einsum('bcshk,bcshv->bchkv', k, v_w)
    chunk_decay = jnp.exp(Lc * log_gamma)

    def step(carry, s_c):
        return carry * chunk_decay[None, :, None, None] + s_c, carry

    init = jnp.zeros((Bsz, H, Dh, Dh), dtype=chunk_states.dtype)
    _, prev = lax.scan(step, init, jnp.moveaxis(chunk_states, 1, 0))
    prev = jnp.moveaxis(prev, 0, 1)
    w_query = jnp.exp((idx + 1.0)[None, :] * log_gamma[:, None])
    y_cross = jnp.einsum('bclhk,bchkv->bclhv', q, prev) * w_query.T[None, None, :, :, None]
    return (y_intra + y_cross).reshape(Bsz, T, H, Dh)


def causal_conv(u, w, b):
    K = w.shape[0]
    T = u.shape[1]
    up = jnp.pad(u, ((0, 0), (K - 1, 0), (0, 0)))
    out = b[None, None, :]
    for i in range(K):
        out = out + up[:, i:i + T, :] * w[i]
    return out


def ssd(xs, dt, a_log, bm, cm, d_skip):
    Bsz, T, H, P = xs.shape
    G, N = bm.shape[-2], bm.shape[-1]
    J = H // G
    Lc = SSM_CHUNK
    C = T // Lc
    a = dt * (-jnp.exp(a_log))
    x_dt = (xs * dt[..., None]).reshape(Bsz, C, Lc, G, J, P)
    b = bm.reshape(Bsz, C, Lc, G, N)
    c = cm.reshape(Bsz, C, Lc, G, N)
    a_cs = jnp.cumsum(a.reshape(Bsz, C, Lc, H), axis=2)
    causal = jnp.tril(jnp.ones((Lc, Lc), dtype=bool))
    seg = a_cs[:, :, :, None, :] - a_cs[:, :, None, :, :]
    decay = jnp.exp(jnp.where(causal[None, None, :, :, None], seg, -jnp.inf)).reshape(Bsz, C, Lc, Lc, G, J)
    cb = jnp.einsum('bclgn,bcsgn->bclsg', c, b)
    y_diag = jnp.einsum('bclsgj,bcsgjp->bclgjp', cb[..., None] * decay, x_dt)
    to_end = jnp.exp(a_cs[:, :, -1:, :] - a_cs).reshape(Bsz, C, Lc, G, J)
    chunk_states = jnp.einsum('bcsgn,bcsgjp->bcgjpn', b, x_dt * to_end[..., None])
    chunk_decay = jnp.exp(a_cs[:, :, -1, :]).reshape(Bsz, C, G, J)

    def step(carry, inp):
        s_c, d_c = inp
        return carry * d_c[..., None, None] + s_c, carry

    init = jnp.zeros((Bsz, G, J, P, N), dtype=chunk_states.dtype)
    _, prev = lax.scan(step, init, (jnp.moveaxis(chunk_states, 1, 0), jnp.moveaxis(chunk_decay, 1, 0)))
    prev = jnp.moveaxis(prev, 0, 1)
    from_start = jnp.exp(a_cs).reshape(Bsz, C, Lc, G, J)
    y_off = jnp.einsum('bclgn,bcgjpn->bclgjp', c, prev) * from_start[..., None]
    y = (y_diag + y_off).reshape(Bsz, T, H, P)
    return y + xs * d_skip[:, None]


def token_mixer(h, positions, w_in, conv_w, conv_b, dt_bias, a_log, d_skip, ret_norm_w, ssm_norm_w, w_out):
    Bsz, T, _ = h.shape
    f32 = jnp.float32
    proj = jnp.einsum('btd,de->bte', h, w_in).astype(f32)
    cuts = [RET_WIDTH, 2 * RET_WIDTH, 3 * RET_WIDTH, 4 * RET_WIDTH,
            4 * RET_WIDTH + SSM_WIDTH, 4 * RET_WIDTH + SSM_WIDTH + SSM_CONV_DIM]
    q, k, v, g, z, xbc, dt_raw = jnp.split(proj, cuts, axis=-1)
    hs = (Bsz, T, RET_HEADS, RET_HEAD_DIM)
    y_ret = retention(q.reshape(hs), k.reshape(hs), v.reshape(hs), positions)
    mu = jnp.mean(y_ret, axis=-1, keepdims=True)
    var = jnp.mean(jnp.square(y_ret - mu), axis=-1, keepdims=True)
    y_ret = ((y_ret - mu) * lax.rsqrt(var + EPS)).reshape(Bsz, T, RET_WIDTH)
    y_ret = y_ret * ret_norm_w.astype(f32) * jax.nn.silu(g)
    xbc = jax.nn.silu(causal_conv(xbc, conv_w.astype(f32), conv_b.astype(f32)))
    xs, bm, cm = jnp.split(xbc, [SSM_WIDTH, SSM_WIDTH + SSM_GROUPS * SSM_STATE], axis=-1)
    dt = jax.nn.softplus(dt_raw + dt_bias.astype(f32))
    y_ssm = ssd(xs.reshape(Bsz, T, SSM_HEADS, SSM_HEAD_DIM), dt, a_log.astype(f32),
                bm.reshape(Bsz, T, SSM_GROUPS, SSM_STATE), cm.reshape(Bsz, T, SSM_GROUPS, SSM_STATE),
                d_skip.astype(f32))
    y_ssm = y_ssm.reshape(Bsz, T, SSM_WIDTH) * jax.nn.silu(z)
    y_ssm = y_ssm.reshape(Bsz, T, SSM_GROUPS, SSM_WIDTH // SSM_GROUPS)
    y_ssm = y_ssm * lax.rsqrt(jnp.mean(y_ssm * y_ssm, axis=-1, keepdims=True) + EPS)
    y_ssm = y_ssm.reshape(Bsz, T, SSM_WIDTH) * ssm_norm_w.astype(f32)
    y = jnp.concatenate([y_ret, y_ssm], axis=-1).astype(h.dtype)
    return jnp.einsum('bte,ed->btd', y, w_out)


def hier_moe(h, w_rg, b_rg, w_re, b_re, w_gate, w_up, w_down):
    Bsz, T, D = h.shape
    ht = h.reshape(Bsz * T, D)
    f32 = jnp.float32
    logits_g = (ht @ w_rg).astype(f32) + b_rg.astype(f32)
    p_g = jax.nn.softmax(logits_g, axis=-1)
    g_sel = jnp.argmax(logits_g, axis=-1)
    oh_g = jax.nn.one_hot(g_sel, N_EXPERT_GROUPS, dtype=f32)
    p_sel = jnp.sum(p_g * oh_g, axis=-1)
    logits_e = ((ht @ w_re).astype(f32) + b_re.astype(f32)).reshape(-1, N_EXPERT_GROUPS, EXPERTS_PER_GROUP)
    le_sel = jnp.einsum('tge,tg->te', logits_e, oh_g)
    p_e = jax.nn.softmax(le_sel, axis=-1)
    top_w, top_i = lax.top_k(p_e, TOP_K)
    top_w = top_w / jnp.sum(top_w, axis=-1, keepdims=True)
    w_in_group = jnp.sum(jax.nn.one_hot(top_i, EXPERTS_PER_GROUP, dtype=f32) * top_w[..., None], axis=1)
    comb = oh_g[:, :, None] * w_in_group[:, None, :] * p_sel[:, None, None]
    out = jnp.zeros((Bsz * T, D), dtype=f32)
    for grp in range(N_EXPERT_GROUPS):
        sl = slice(grp * EXPERTS_PER_GROUP, (grp + 1) * EXPERTS_PER_GROUP)
        a = jnp.einsum('td,edf->tef', ht, w_gate[sl])
        u = jnp.einsum('td,edf->tef', ht, w_up[sl])
        act = (jax.nn.silu(a.astype(f32)) * u.astype(f32) * comb[:, grp, :, None]).astype(h.dtype)
        out = out + jnp.einsum('tef,efd->td', act, w_down[sl]).astype(f32)
    return out.astype(h.dtype).reshape(Bsz, T, D)


def setup_inputs(seed: int = 0) -> dict:
    key = jax.random.key(seed)
    ks = jax.random.split(key, 24)
    L = DEPTH
    nrm = jax.random.normal
    x = nrm(ks[0], (BATCH, SEQ, D_MODEL), jnp.float32)
    positions = jnp.broadcast_to(jnp.arange(SEQ, dtype=jnp.int32)[None, :], (BATCH, SEQ))
    norm1_w = 1.0 + 0.02 * nrm(ks[1], (L, D_MODEL), jnp.float32)
    w_in = nrm(ks[2], (L, D_MODEL, IN_PROJ_DIM), jnp.float32) * (D_MODEL ** -0.5)
    conv_w = nrm(ks[3], (L, SSM_CONV, SSM_CONV_DIM), jnp.float32) * (SSM_CONV ** -0.5)
    conv_b = 0.01 * nrm(ks[4], (L, SSM_CONV_DIM), jnp.float32)
    dt0 = jnp.exp(jax.random.uniform(ks[5], (L, SSM_HEADS), jnp.float32, math.log(1e-3), math.log(1e-1)))
    dt_bias = dt0 + jnp.log(-jnp.expm1(-dt0))
    a_log = jnp.log(jax.random.uniform(ks[6], (L, SSM_HEADS), jnp.float32, 1.0, 16.0))
    d_skip = 1.0 + 0.1 * nrm(ks[7], (L, SSM_HEADS), jnp.float32)
    ret_norm_w = 1.0 + 0.02 * nrm(ks[8], (L, RET_WIDTH), jnp.float32)
    ssm_norm_w = 1.0 + 0.02 * nrm(ks[9], (L, SSM_WIDTH), jnp.float32)
    w_out = nrm(ks[10], (L, D_MIX, D_MODEL), jnp.float32) * (D_MIX ** -0.5)
    norm2_w = 1.0 + 0.02 * nrm(ks[11], (L, D_MODEL), jnp.float32)
    w_router_group = nrm(ks[12], (L, D_MODEL, N_EXPERT_GROUPS), jnp.float32) * (D_MODEL ** -0.5)
    b_router_group = 0.01 * nrm(ks[13], (L, N_EXPERT_GROUPS), jnp.float32)
    w_router_expert = nrm(ks[14], (L, D_MODEL, N_EXPERTS), jnp.float32) * (D_MODEL ** -0.5)
    b_router_expert = 0.01 * nrm(ks[15], (L, N_EXPERTS), jnp.float32)
    w_expert_gate = nrm(ks[16], (L, N_EXPERTS, D_MODEL, D_EXPERT), jnp.float32) * (D_MODEL ** -0.5)
    w_expert_up = nrm(ks[17], (L, N_EXPERTS, D_MODEL, D_EXPERT), jnp.float32) * (D_MODEL ** -0.5)
    w_expert_down = nrm(ks[18], (L, N_EXPERTS, D_EXPERT, D_MODEL), jnp.float32) * (D_EXPERT ** -0.5)
    final_norm_w = 1.0 + 0.02 * nrm(ks[19], (D_MODEL,), jnp.float32)
    return {'x': x, 'positions': positions, 'norm1_w': norm1_w, 'w_in': w_in,
            'conv_w': conv_w, 'conv_b': conv_b, 'dt_bias': dt_bias, 'a_log': a_log,
            'd_skip': d_skip, 'ret_norm_w': ret_norm_w, 'ssm_norm_w': ssm_norm_w,
            'w_out': w_out, 'norm2_w': norm2_w, 'w_router_group': w_router_group,
            'b_router_group': b_router_group, 'w_router_expert': w_router_expert,
            'b_router_expert': b_router_expert, 'w_expert_gate': w_expert_gate,
            'w_expert_up': w_expert_up, 'w_expert_down': w_expert_down,
            'final_norm_w': final_norm_w}


def reference(x, positions, norm1_w, w_in, conv_w, conv_b, dt_bias, a_log, d_skip,
              ret_norm_w, ssm_norm_w, w_out, norm2_w, w_router_group, b_router_group,
              w_router_expert, b_router_expert, w_expert_gate, w_expert_up, w_expert_down,
              final_norm_w):
    h = x
    for layer in range(DEPTH):
        h = h + token_mixer(rmsnorm(h, norm1_w[layer]), positions, w_in[layer], conv_w[layer],
                            conv_b[layer], dt_bias[layer], a_log[layer], d_skip[layer],
                            ret_norm_w[layer], ssm_norm_w[layer], w_out[layer])
        h = h + hier_moe(rmsnorm(h, norm2_w[layer]), w_router_group[layer], b_router_group[layer],
                         w_router_expert[layer], b_router_expert[layer], w_expert_gate[layer],
                         w_expert_up[layer], w_expert_down[layer])
    return rmsnorm(h, final_norm_w)
```

```python
import contextlib
import numpy as np
import concourse.bass as bass
import concourse.mybir as mybir
from concourse.bass_utils import run_bass_kernel_spmd

F32 = mybir.dt.float32
BF16 = mybir.dt.bfloat16
I32 = mybir.dt.int32
AF = mybir.ActivationFunctionType
ALU = mybir.AluOpType
ENGS = ("pe", "act", "dve", "pool", "sp")
NCORE = 8
D = 2048
SEQ = 8192
TOK = SEQ // NCORE
NCH = SEQ // 128
OWN0 = NCH - TOK // 128
INP = 6672
EPS = 1e-6


class Tok:
    __slots__ = ("name", "writer", "readers")

    def __init__(self, name):
        self.name = name
        self.writer = None
        self.readers = []


class Op:
    __slots__ = ("eng", "fn", "deps", "dma_sem", "dma_val", "signal", "sig_val")


class Sched:
    def __init__(self, nc):
        self.nc = nc
        self.ops = {e: [] for e in ENGS}
        self.dma_counts = {}
        self.tk = {}

    def T(self, name):
        t = self.tk.get(name)
        if t is None:
            t = self.tk[name] = Tok(name)
        return t

    def add(self, eng, fn, reads=(), writes=(), dma=None):
        op = Op()
        op.eng, op.fn, op.deps, op.signal, op.sig_val = eng, fn, [], False, None
        op.dma_sem, op.dma_val = dma, None
        if dma is not None:
            c = self.dma_counts.get(dma, 0) + 16
            self.dma_counts[dma] = c
            op.dma_val = c
        deps = {}
        rt = [self.T(x) for x in reads]
        wt = [self.T(x) for x in writes]
        for t in rt:
            if t.writer is not None:
                deps[id(t.writer)] = t.writer
        for t in wt:
            if t.writer is not None:
                deps[id(t.writer)] = t.writer
            for r in t.readers:
                deps[id(r)] = r
        for d in deps.values():
            if d.dma_sem is None and d.eng == "pe" and eng == "pe" and dma is None:
                continue
            op.deps.append(d)
            if d.dma_sem is None:
                d.signal = True
        for t in rt:
            t.readers.append(op)
        for t in wt:
            t.writer = op
            t.readers = []
        self.ops[eng].append(op)
        return op

    def group_close(self, dma):
        tot = self.dma_counts[dma]
        for e in ENGS:
            for op in self.ops[e]:
                if op.dma_sem == dma:
                    op.dma_val = tot

    def emit(self):
        nc = self.nc
        with contextlib.ExitStack() as stack:
            block = stack.enter_context(nc.Block())
            esem = {e: stack.enter_context(nc.semaphore(f"s_{e}")) for e in ENGS if e != "sp"}
            dsem = {k: stack.enter_context(nc.semaphore(f"d_{k}")) for k in self.dma_counts}
            for e in ENGS:
                c = 0
                for op in self.ops[e]:
                    if op.dma_sem is None and op.signal:
                        c += 1
                        op.sig_val = c
            ops = self.ops
            counts = self.dma_counts

            def run(e, engobj):
                waited = {}
                for op in ops[e]:
                    for d in op.deps:
                        if d.dma_sem is not None:
                            key, sem, val = ("d", d.dma_sem), dsem[d.dma_sem], d.dma_val
                        else:
                            key, sem, val = ("e", d.eng), esem[d.eng], d.sig_val
                        if waited.get(key, 0) >= val:
                            continue
                        waited[key] = val
                        engobj.wait_ge(sem, val)
                    ins = op.fn(engobj)
                    if op.dma_sem is not None:
                        ins.then_inc(dsem[op.dma_sem], 16)
                    elif op.signal:
                        ins.then_inc(esem[e], 1)
                if e == "sp":
                    for k, c in counts.items():
                        engobj.wait_ge(dsem[k], c)

            @block.tensor
            def _(pe):
                run("pe", pe)

            @block.scalar
            def _(act):
                run("act", act)

            @block.vector
            def _(dve):
                run("dve", dve)

            @block.gpsimd
            def _(pool):
                run("pool", pool)

            @block.sync
            def _(sp):
                run("sp", sp)


def build_nc(chunks=None, upto=99, dump=None):
    nc = bass.Bass("TRN2", target_bir_lowering=False)
    DI = lambda n, sh, dt=F32: nc.dram_tensor(n, sh, dt, kind="ExternalInput").ap()
    x_d = DI("x", [SEQ, D])
    pos_d = DI("pos", [1, SEQ], I32)
    valid_d = DI("valid", [128, NCH])
    g1_d = DI("norm1_w", [1, D])
    win_d = DI("w_in", [D, INP])
    cw_d = DI("conv_w", [128, 12, 4])
    cb_d = DI("conv_b", [128, 12])
    dtb_d = DI("dt_bias", [1, 16])
    alog_d = DI("a_log", [1, 16])
    dsk_d = DI("d_skip", [1, 16])
    rnw_d = DI("ret_norm_w", [1, 1024])
    snw_d = DI("ssm_norm_w", [1, 1024])
    wout_d = DI("w_out", [D, D])
    g2_d = DI("norm2_w", [1, D])
    wr_d = DI("w_router", [D, 36])
    rb_d = DI("b_router", [1, 36])
    wg_d = DI("w_gate", [32, D, 512])
    wu_d = DI("w_up", [32, D, 512])
    wd_d = DI("w_down", [32, 512, D])
    gf_d = DI("final_norm_w", [1, D])
    ident_d = DI("ident", [128, 128])
    caus_d = DI("caus", [128, 128])
    U_d = DI("U", [128, 128])
    negm_d = DI("negm", [128, 128])
    rq_d = DI("rq", [1, 512])
    rk_d = DI("rk", [1, 512])
    G_d = DI("G", [1, 4])
    inv_d = DI("inv", [128, 1])
    out_d = nc.dram_tensor("out", [TOK, D], F32, kind="ExternalOutput").ap()
    dbg_d = nc.dram_tensor("dbg", [128, D], F32, kind="ExternalOutput").ap() if dump else None

    s = Sched(nc)
    stack = contextlib.ExitStack()
    SB = lambda n, sh, dt=F32: stack.enter_context(nc.sbuf_tensor("sb_" + n, sh, dt))
    PI = float(np.pi)

    xin = SB("xin", [128, D]); junk = SB("junk", [128, D], BF16); xn = SB("xn", [128, D], BF16)
    g1 = SB("g1", [128, D]); g2 = SB("g2", [128, D]); gf = SB("gf", [128, D])
    rnw = SB("rnw", [128, 1024]); snw = SB("snw", [128, 1024])
    ss = SB("ss", [128, 1]); rstd = SB("rstd", [128, 1])
    xnT = SB("xnT", [128, 16, 128], BF16)
    hbuf = SB("h", [128, D])
    identf = SB("identf", [128, 128]); identb = SB("identb", [128, 128], BF16)
    causb = SB("causb", [128, 128], BF16); causf = SB("causf", [128, 128])
    U = SB("U", [128, 128]); negm = SB("negm", [128, 128]); ones = SB("ones", [128, 128])
    rq = SB("rq", [128, 4, 128]); rk = SB("rk", [128, 4, 128]); Gb = SB("Gb", [128, 4])
    inv = SB("inv", [128, 1]); valid = SB("valid", [128, NCH])
    cw = SB("cw", [128, 12, 4]); cb = SB("cb", [128, 12])
    dtb = SB("dtb", [128, 16]); negA = SB("negA", [128, 16]); dsk = SB("dsk", [128, 16])
    wr = SB("wr", [128, 16, 36]); rb = SB("rb", [128, 36])
    posi = SB("posi", [128, 128], I32); pf = SB("pf", [128, 128]); ang = SB("ang", [128, 128])
    ra = SB("ra", [128, 128]); ki = SB("ki", [128, 128], I32); kf = SB("kf", [128, 128]); mk = SB("mk", [128, 128])
    cos = SB("cos", [128, 128]); sin = SB("sin", [128, 128])
    rt = [SB(f"rt{i}", [128, 128]) for i in range(6)]
    qT = SB("qT", [128, 8, 128], BF16); kT = SB("kT", [128, 8, 128], BF16)
    v = SB("v", [128, 1024], BF16); gs = SB("gs", [128, 1024]); zs = SB("zs", [128, 1024])
    xbc = SB("xbc", [128, 12, 131]); xc = SB("xc", [128, 12, 128], BF16)
    cvo = [SB(f"cvo{i}", [128, 128]) for i in range(2)]
    dtr = SB("dtr", [128, 16]); dt = SB("dt", [128, 16]); dtv = SB("dtv", [128, 16]); aa = SB("aa", [128, 16])
    acs = SB("acs", [128, 16]); nacs = SB("nacs", [128, 16]); cdec = SB("cdec", [128, 16]); te = SB("te", [128, 16]); el = SB("el", [128, 16])
    tmp16 = SB("tmp16", [128, 16])
    ktok = SB("ktok", [128, 1024], BF16); xstok = SB("xstok", [128, 1024], BF16); Btok = SB("Btok", [128, 2, 128], BF16)
    scm = SB("scm", [128, 128], BF16)
    st6 = SB("st6", [128, 6]); mv = SB("mv", [128, 2]); rs = SB("rs", [128, 1])
    yn = SB("yn", [128, 256]); ycat = SB("ycat", [128, D]); ycb = xn
    S = SB("S", [128, 4, 512]); Sb = SB("Sb", [128, 4, 512], BF16)
    H = SB("H", [128, 1024]); Hb = SB("Hb", [128, 1024], BF16)
    xdt = SB("xdt", [128, 1024], BF16); xdtw = SB("xdtw", [128, 1024], BF16)
    cbt = SB("cbt", [128, 2, 128]); abc = SB("abc", [128, 128]); dec = SB("dec", [128, 128]); Mb = SB("Mb", [128, 128], BF16)
    ty = SB("ty", [128, 1024]); tu = SB("tu", [128, 512]); ss2 = SB("ss2", [128, 1]); r2 = SB("r2", [128, 1])
    yT = SB("yT", [128, 16, 128], BF16)
    hn = ycat; lob = SB("lob", [128, D], BF16); loT = SB("loT", [128, 16, 128], BF16); hnT = SB("hnT", [128, 16, 128], BF16)
    wrhi = SB("wrhi", [128, 16, 36], BF16); wrlo = SB("wrlo", [128, 16, 36], BF16)
    lg = SB("lg", [128, 36]); mx = SB("mx", [128, 1]); nmx = SB("nmx", [128, 1]); ohg = SB("ohg", [128, 4])
    eg = SB("eg", [128, 4]); sg = SB("sg", [128, 1]); psel = SB("psel", [128, 1])
    les = SB("les", [128, 8]); les2 = SB("les2", [128, 8]); k1 = SB("k1", [128, 8]); k2 = SB("k2", [128, 8])
    m1 = SB("m1", [128, 1]); m2 = SB("m2", [128, 1]); dd = SB("dd", [128, 1]); ed = SB("ed", [128, 1]); w1 = SB("w1", [128, 1]); w2 = SB("w2", [128, 1])
    wig = SB("wig", [128, 8]); scg = SB("scg", [128, 1]); comb = SB("comb", [128, 32])
    sgl = SB("sgl", [128, 512]); actb = SB("actb", [128, 512], BF16); actT = SB("actT", [128, 4, 128], BF16)
    obuf = ycat
    NW = 2
    wts = [SB(f"wt{i}", [128, 16 * 512], BF16) for i in range(NW)]
    pg = [stack.enter_context(nc.psum_tensor(f"pg{i}", [128, 512], F32)) for i in range(6)]
    pt = [stack.enter_context(nc.psum_tensor(f"pt{i}", [128, 1024], BF16)) for i in range(2)]
    pcnt = [0]

    def nextp():
        i = pcnt[0] % 4
        pcnt[0] += 1
        return pg[i], f"pg{i}"

    wcnt = [0]

    def wload(src_ap, view):
        i = wcnt[0] % NW
        wcnt[0] += 1
        buf = wts[i]
        s.add("pool", lambda e: e.dma_start(out=view(buf), in_=src_ap), writes=[f"wt{i}"], dma=f"w{i}")
        return buf, f"wt{i}"

    A = s.add

    def DUMP(name, ap, tok, width):
        if dump == name:
            A("pool", lambda e: e.dma_start(out=dbg_d[:, 0:width], in_=ap), [tok], ["dbgd"], dma="dbg")
    bc = lambda ap, sh: ap.to_broadcast(sh)

    def cload(buf_ap, src, tok):
        A("sp", lambda e: e.dma_start(out=buf_ap, in_=src), writes=[tok], dma="const")
    cload(g1[:, :], g1_d[0:1, :].partition_broadcast(128), "g1")
    cload(g2[:, :], g2_d[0:1, :].partition_broadcast(128), "g2")
    cload(gf[:, :], gf_d[0:1, :].partition_broadcast(128), "gf")
    cload(rnw[:, :], rnw_d[0:1, :].partition_broadcast(128), "rnw")
    cload(snw[:, :], snw_d[0:1, :].partition_broadcast(128), "snw")
    cload(identf[:, :], ident_d[:, :], "identf")
    cload(causf[:, :], caus_d[:, :], "causf")
    cload(U[:, :], U_d[:, :], "U")
    cload(negm[:, :], negm_d[:, :], "negm")
    cload(rq[:, :, :].rearrange("p h l -> p (h l)"), rq_d[0:1, :].partition_broadcast(128), "rq")
    cload(rk[:, :, :].rearrange("p h l -> p (h l)"), rk_d[0:1, :].partition_broadcast(128), "rk")
    cload(Gb[:, :], G_d[0:1, :].partition_broadcast(128), "Gb")
    cload(inv[:, :], inv_d[:, :], "inv")
    cload(valid[:, :], valid_d[:, :], "valid")
    cload(cw[:, :, :], cw_d[:, :, :], "cw")
    cload(cb[:, :], cb_d[:, :], "cb")
    cload(dtb[:, :], dtb_d[0:1, :].partition_broadcast(128), "dtb")
    cload(negA[:, :], alog_d[0:1, :].partition_broadcast(128), "negA")
    cload(dsk[:, :], dsk_d[0:1, :].partition_broadcast(128), "dsk")
    cload(wr[:, :, :], wr_d.rearrange("(k p) n -> p k n", p=128), "wr")
    cload(rb[:, :], rb_d[0:1, :].partition_broadcast(128), "rb")
    s.group_close("const")
    A("dve", lambda e: e.tensor_copy(out=identb[:, :], in_=identf[:, :]), ["identf"], ["identb"])
    A("dve", lambda e: e.tensor_copy(out=causb[:, :], in_=causf[:, :]), ["causf"], ["causb"])
    A("dve", lambda e: e.memset(ones[:, :], 1.0), [], ["ones"])
    A("dve", lambda e: e.tensor_copy(out=wrhi[:, :, :], in_=wr[:, :, :]), ["wr"], ["wrhi"])
    A("dve", lambda e: e.tensor_tensor(out=wrlo[:, :, :], in0=wr[:, :, :], in1=wrhi[:, :, :], op=ALU.subtract), ["wr", "wrhi"], ["wrlo"])
    A("act", lambda e: e.activation(out=negA[:, :], in_=negA[:, :], func=AF.Exp), ["negA"], ["negA"])
    A("dve", lambda e: e.tensor_scalar(out=negA[:, :], in0=negA[:, :], scalar1=-1.0, scalar2=None, op0=ALU.mult), ["negA"], ["negA"])
    A("dve", lambda e: e.memset(xbc[:, :, :], 0.0), [], ["xbc"])
    A("dve", lambda e: e.memset(S[:, :, :], 0.0), [], ["S"])
    A("dve", lambda e: e.memset(Sb[:, :, :], 0.0), [], ["Sb"])
    A("dve", lambda e: e.memset(H[:, :], 0.0), [], ["H"])
    A("dve", lambda e: e.memset(Hb[:, :], 0.0), [], ["Hb"])

    def rms(src, srctok, wbuf, wtok, dst, dsttok):
        A("act", lambda e: e.activation(out=junk[:, :], in_=src[:, :], func=AF.Square, accum_out=ss[:, 0:1]), [srctok], ["ss", "junk"])
        A("act", lambda e: e.activation(out=rstd[:, :], in_=ss[:, :], func=AF.Sqrt, scale=1.0 / D, bias=EPS), ["ss"], ["rstd"])
        A("dve", lambda e: e.reciprocal(out=rstd[:, :], in_=rstd[:, :]), ["rstd"], ["rstd"])
        A("dve", lambda e: e.scalar_tensor_tensor(out=dst[:, :], in0=src[:, :], scalar=rstd[:, 0:1], in1=wbuf[:, :], op0=ALU.mult, op1=ALU.mult), [srctok, "rstd", wtok], [dsttok])

    def transposes16(src, srctok, dst, dsttok):
        for half in range(2):
            p = pt[half]
            def f(e, half=half, p=p):
                for k in range(8):
                    kk = half * 8 + k
                    i = e.transpose(out=p[:, k * 128:(k + 1) * 128], in_=src[:, kk * 128:(kk + 1) * 128], identity=identb[:, :])
                return i
            A("pe", f, [srctok, "identb"], [f"pt{half}"])
            eng = "act" if half == 0 else "dve"
            if eng == "act":
                A("act", lambda e, half=half, p=p: e.copy(out=dst[:, half * 8:(half + 1) * 8, :], in_=p[:, :].rearrange("p (k t) -> p k t", k=8)), [f"pt{half}"], [dsttok])
            else:
                A("dve", lambda e, half=half, p=p: e.tensor_copy(out=dst[:, half * 8:(half + 1) * 8, :], in_=p[:, :].rearrange("p (k t) -> p k t", k=8)), [f"pt{half}"], [dsttok])

    def sincos(shift, outbuf, outtok):
        A("dve", lambda e: e.tensor_scalar(out=ra[:, :], in0=ang[:, :], scalar1=float(shift), scalar2=None, op0=ALU.add), ["ang"], ["ra"])
        A("dve", lambda e: e.tensor_scalar(out=ki[:, :], in0=ra[:, :], scalar1=float(1 / (2 * PI)), scalar2=None, op0=ALU.mult), ["ra"], ["ki"])
        A("dve", lambda e: e.tensor_copy(out=kf[:, :], in_=ki[:, :]), ["ki"], ["kf"])
        A("dve", lambda e: e.scalar_tensor_tensor(out=ra[:, :], in0=kf[:, :], scalar=float(-2 * PI), in1=ra[:, :], op0=ALU.mult, op1=ALU.add), ["kf", "ra"], ["ra"])
        A("dve", lambda e: e.tensor_scalar(out=mk[:, :], in0=ra[:, :], scalar1=PI, scalar2=float(-2 * PI), op0=ALU.is_gt, op1=ALU.mult), ["ra"], ["mk"])
        A("dve", lambda e: e.tensor_tensor(out=ra[:, :], in0=ra[:, :], in1=mk[:, :], op=ALU.add), ["ra", "mk"], ["ra"])
        A("act", lambda e: e.activation(out=outbuf[:, :], in_=ra[:, :], func=AF.Sin), ["ra"], [outtok])

    v3 = lambda ap, h: ap.rearrange("p (h j) -> p h j", h=h)

    for c in (range(NCH) if chunks is None else chunks):
        own = c >= OWN0
        cs = slice(c * 128, (c + 1) * 128)
        A("sp", lambda e, cs=cs: e.dma_start(out=xin[:, :], in_=x_d[cs, :]), [], ["xin"], dma="x")
        A("sp", lambda e, cs=cs: e.dma_start(out=posi[:, :], in_=pos_d[0:1, cs].partition_broadcast(128)), [], ["posi"], dma="pos")
        rms(xin, "xin", g1, "g1", xn, "xn")
        if own:
            A("pool", lambda e: e.tensor_copy(out=hbuf[:, :], in_=xin[:, :]), ["xin"], ["h"])
        transposes16(xn, "xn", xnT, "xnT")
        A("dve", lambda e: e.tensor_copy(out=pf[:, :], in_=posi[:, :]), ["posi"], ["pf"])
        A("dve", lambda e: e.tensor_scalar(out=ang[:, :], in0=pf[:, :], scalar1=inv[:, 0:1], scalar2=None, op0=ALU.mult), ["pf", "inv"], ["ang"])
        sincos(0.0, sin, "sin")
        sincos(PI / 2, cos, "cos")

        if upto <= 1:
            break
        DUMP("xn", xn[:, :], "xn", 2048)
        DUMP("cos", cos[:, :], "cos", 128)
        DUMP("sin", sin[:, :], "sin", 128)
        tiles = list(range(14)) if own else [2, 3, 4, 5, 10, 11, 12, 13]
        for t in tiles:
            wcols = 512 if t < 13 else 16
            wb, wtok = wload(win_d[:, t * 512:t * 512 + wcols].rearrange("(k p) n -> p k n", p=128),
                             lambda b, wcols=wcols: b[:, :].rearrange("p (k n) -> p k n", k=16)[:, :, 0:wcols])
            w3 = wb[:, :].rearrange("p (k n) -> p k n", k=16)
            if t in (0, 1, 2, 3, 10, 11, 12):
                p, ptok = nextp()
                def f(e, p=p, w3=w3):
                    for b in range(4):
                        for k in range(16):
                            i = e.matmul(p[:, b * 128:(b + 1) * 128], lhsT=w3[:, k, b * 128:(b + 1) * 128], rhs=xnT[:, k, :], start=(k == 0), stop=(k == 15))
                    return i
                A("pe", f, [wtok, "xnT"], [ptok])
                if t >= 10:
                    A("act", lambda e, p=p, t=t: e.copy(out=xbc[:, (t - 10) * 4:(t - 10) * 4 + 4, 3:131], in_=p[:, :].rearrange("p (b t) -> p b t", b=4)), [ptok], ["xbc"])
                else:
                    isq = t < 2
                    dstT, dtok, tbl, tbtok = (qT, "qT", rq, "rq") if isq else (kT, "kT", rk, "rk")
                    for hh in range(2):
                        h = (t % 2) * 2 + hh
                        x1 = p[:, (2 * hh) * 128:(2 * hh + 1) * 128]
                        x2 = p[:, (2 * hh + 1) * 128:(2 * hh + 2) * 128]
                        A("dve", lambda e, x1=x1: e.tensor_tensor(out=rt[0][:, :], in0=x1, in1=cos[:, :], op=ALU.mult), [ptok, "cos"], ["rt0"])
                        A("dve", lambda e, x2=x2: e.tensor_tensor(out=rt[1][:, :], in0=x2, in1=sin[:, :], op=ALU.mult), [ptok, "sin"], ["rt1"])
                        A("dve", lambda e, x2=x2: e.tensor_tensor(out=rt[2][:, :], in0=x2, in1=cos[:, :], op=ALU.mult), [ptok, "cos"], ["rt2"])
                        A("dve", lambda e, x1=x1: e.tensor_tensor(out=rt[3][:, :], in0=x1, in1=sin[:, :], op=ALU.mult), [ptok, "sin"], ["rt3"])
                        A("pool", lambda e: e.tensor_tensor(out=rt[4][:, :], in0=rt[0][:, :], in1=rt[1][:, :], op=ALU.subtract), ["rt0", "rt1"], ["rt4"])
                        A("pool", lambda e: e.tensor_tensor(out=rt[5][:, :], in0=rt[2][:, :], in1=rt[3][:, :], op=ALU.add), ["rt2", "rt3"], ["rt5"])
                        A("pool", lambda e, h=h, dstT=dstT, tbl=tbl: e.tensor_tensor(out=dstT[:, 2 * h, :], in0=rt[4][:, :], in1=tbl[:, h, :], op=ALU.mult), ["rt4", tbtok], [dtok])
                        A("pool", lambda e, h=h, dstT=dstT, tbl=tbl: e.tensor_tensor(out=dstT[:, 2 * h + 1, :], in0=rt[5][:, :], in1=tbl[:, h, :], op=ALU.mult), ["rt5", tbtok], [dtok])
            else:
                p, ptok = nextp()
                def f(e, p=p, w3=w3, wcols=wcols):
                    for k in range(16):
                        i = e.matmul(p[:, 0:wcols], lhsT=xnT[:, k, :], rhs=w3[:, k, 0:wcols], start=(k == 0), stop=(k == 15))
                    return i
                A("pe", f, [wtok, "xnT"], [ptok])
                if t in (4, 5):
                    A("act", lambda e, p=p, t=t: e.copy(out=v[:, (t - 4) * 512:(t - 3) * 512], in_=p[:, :]), [ptok], ["v"])
                elif t in (6, 7):
                    A("act", lambda e, p=p, t=t: e.activation(out=gs[:, (t - 6) * 512:(t - 5) * 512], in_=p[:, :], func=AF.Silu), [ptok], ["gs"])
                elif t in (8, 9):
                    A("act", lambda e, p=p, t=t: e.activation(out=zs[:, (t - 8) * 512:(t - 7) * 512], in_=p[:, :], func=AF.Silu), [ptok], ["zs"])
                else:
                    A("dve", lambda e, p=p: e.tensor_tensor(out=dtr[:, :], in0=p[:, 0:16], in1=dtb[:, :], op=ALU.add), [ptok, "dtb"], ["dtr"])
                    A("act", lambda e: e.activation(out=dtr[:, :], in_=dtr[:, :], func=AF.Exp), ["dtr"], ["dtr"])
                    A("act", lambda e: e.activation(out=dt[:, :], in_=dtr[:, :], func=AF.Ln, bias=1.0), ["dtr"], ["dt"])
        if upto <= 2:
            break
        for blk in range(12):
            o = cvo[blk % 2]
            otok = f"cvo{blk % 2}"
            A("dve", lambda e, o=o, blk=blk: e.tensor_scalar(out=o[:, :], in0=xbc[:, blk, 0:128], scalar1=cw[:, blk, 0:1], scalar2=None, op0=ALU.mult), ["xbc", "cw"], [otok])
            for i in (1, 2, 3):
                A("dve", lambda e, o=o, blk=blk, i=i: e.scalar_tensor_tensor(out=o[:, :], in0=xbc[:, blk, i:i + 128], scalar=cw[:, blk, i:i + 1], in1=o[:, :], op0=ALU.mult, op1=ALU.add), ["xbc", "cw", otok], [otok])
            A("act", lambda e, o=o, blk=blk: e.activation(out=xc[:, blk, :], in_=o[:, :], func=AF.Silu, bias=cb[:, blk:blk + 1]), [otok, "cb"], ["xc"])
        A("pool", lambda e: e.tensor_copy(out=xbc[:, :, 0:3], in_=xbc[:, :, 128:131]), ["xbc"], ["xbc"])

        if upto <= 3:
            break
        DUMP("qT", qT[:, :, :].rearrange("p a b -> p (a b)"), "qT", 1024)
        DUMP("kT", kT[:, :, :].rearrange("p a b -> p (a b)"), "kT", 1024)
        DUMP("v", v[:, :], "v", 1024)
        DUMP("gs", gs[:, :], "gs", 1024)
        DUMP("xc", xc[:, :, :].rearrange("p a b -> p (a b)"), "xc", 1536)
        DUMP("dt", dt[:, :], "dt", 16)
        def f(e):
            for k in range(8):
                i = e.transpose(out=pt[0][:, k * 128:(k + 1) * 128], in_=kT[:, k, :], identity=identb[:, :])
            return i
        A("pe", f, ["kT", "identb"], ["pt0"])
        A("act", lambda e: e.copy(out=ktok[:, :], in_=pt[0][:, :]), ["pt0"], ["ktok"])
        for h in range(4):
            hs = slice(h * 256, (h + 1) * 256)
            if own:
                p, ptok = nextp()
                def f(e, p=p, h=h):
                    for hf in range(2):
                        i = e.matmul(p[:, 0:128], lhsT=kT[:, 2 * h + hf, :], rhs=qT[:, 2 * h + hf, :], start=(hf == 0), stop=(hf == 1))
                    return i
                A("pe", f, ["kT", "qT"], [ptok])
                A("dve", lambda e, p=p: e.tensor_tensor(out=scm[:, :], in0=p[:, 0:128], in1=causf[:, :], op=ALU.mult), [ptok, "causf"], ["scm"])
                py, pytok = nextp()
                def f(e, py=py, h=h, hs=hs):
                    e.matmul(py[:, 0:256], lhsT=scm[:, :], rhs=v[:, hs], start=True, stop=False)
                    for hf in range(2):
                        i = e.matmul(py[:, 0:256], lhsT=qT[:, 2 * h + hf, :], rhs=Sb[:, h, hf * 256:(hf + 1) * 256], start=False, stop=(hf == 1))
                    return i
                A("pe", f, ["scm", "v", "qT", "Sb"], [pytok])
                A("dve", lambda e, py=py: e.bn_stats(out=st6[:, :], in_=py[:, 0:256]), [pytok], ["st6"])
                A("dve", lambda e: e.bn_aggr(out=mv[:, :], in_=st6[:, :]), ["st6"], ["mv"])
                A("act", lambda e: e.activation(out=rs[:, :], in_=mv[:, 1:2], func=AF.Sqrt, bias=EPS), ["mv"], ["rs"])
                A("dve", lambda e: e.reciprocal(out=rs[:, :], in_=rs[:, :]), ["rs"], ["rs"])
                A("dve", lambda e, py=py: e.tensor_scalar(out=yn[:, :], in0=py[:, 0:256], scalar1=mv[:, 0:1], scalar2=rs[:, 0:1], op0=ALU.subtract, op1=ALU.mult), [pytok, "mv", "rs"], ["yn"])
                A("pool", lambda e, hs=hs: e.tensor_tensor(out=yn[:, :], in0=yn[:, :], in1=rnw[:, hs], op=ALU.mult), ["yn", "rnw"], ["yn"])
                A("pool", lambda e, hs=hs: e.tensor_tensor(out=ycat[:, hs], in0=yn[:, :], in1=gs[:, hs], op=ALU.mult), ["yn", "gs"], ["ycat"])
            pk, pktok = nextp()
            def f(e, pk=pk, h=h, hs=hs):
                for hf in range(2):
                    i = e.matmul(pk[:, hf * 256:(hf + 1) * 256], lhsT=ktok[:, h * 256 + hf * 128:h * 256 + (hf + 1) * 128], rhs=v[:, hs], start=True, stop=True)
                return i
            A("pe", f, ["ktok", "v"], [pktok])
            A("dve", lambda e, pk=pk, h=h: e.tensor_tensor(out=S[:, h, :], in0=pk[:, :], in1=S[:, h, :], op=ALU.add), [pktok, "S"], ["S"])
            A("pool", lambda e, h=h: e.tensor_scalar(out=S[:, h, :], in0=S[:, h, :], scalar1=Gb[:, h:h + 1], scalar2=None, op0=ALU.mult), ["S", "Gb"], ["S"])
            A("pool", lambda e, h=h: e.tensor_copy(out=Sb[:, h, :], in_=S[:, h, :]), ["S"], ["Sb"])

        if upto <= 4:
            break
        A("dve", lambda e, c=c: e.tensor_scalar(out=dtv[:, :], in0=dt[:, :], scalar1=valid[:, c:c + 1], scalar2=None, op0=ALU.mult), ["dt", "valid"], ["dtv"])
        A("dve", lambda e: e.tensor_tensor(out=aa[:, :], in0=dt[:, :], in1=negA[:, :], op=ALU.mult), ["dt", "negA"], ["aa"])
        pm, pmtok = nextp()
        def f(e, pm=pm):
            e.matmul(pm[:, 0:16], lhsT=U[:, :], rhs=aa[:, :], start=True, stop=True)
            return e.matmul(pm[:, 16:32], lhsT=ones[:, :], rhs=aa[:, :], start=True, stop=True)
        A("pe", f, ["U", "ones", "aa"], [pmtok])
        A("act", lambda e, pm=pm: e.copy(out=acs[:, :], in_=pm[:, 0:16]), [pmtok], ["acs"])
        A("dve", lambda e, pm=pm: e.tensor_scalar(out=nacs[:, :], in0=pm[:, 0:16], scalar1=-1.0, scalar2=None, op0=ALU.mult), [pmtok], ["nacs"])
        A("act", lambda e, pm=pm: e.activation(out=cdec[:, :], in_=pm[:, 16:32], func=AF.Exp), [pmtok], ["cdec"])
        A("dve", lambda e, pm=pm: e.tensor_tensor(out=tmp16[:, :], in0=pm[:, 16:32], in1=acs[:, :], op=ALU.subtract), [pmtok, "acs"], ["tmp16"])
        A("act", lambda e: e.activation(out=te[:, :], in_=tmp16[:, :], func=AF.Exp), ["tmp16"], ["te"])
        A("act", lambda e: e.activation(out=el[:, :], in_=acs[:, :], func=AF.Exp), ["acs"], ["el"])
        def f(e):
            for k in range(8):
                i = e.transpose(out=pt[1][:, k * 128:(k + 1) * 128], in_=xc[:, k, :], identity=identb[:, :])
            return i
        A("pe", f, ["xc", "identb"], ["pt1"])
        A("act", lambda e: e.copy(out=xstok[:, :], in_=pt[1][:, :]), ["pt1"], ["xstok"])
        def f(e):
            for g in range(2):
                i = e.transpose(out=pt[0][:, g * 128:(g + 1) * 128], in_=xc[:, 8 + g, :], identity=identb[:, :])
            return i
        A("pe", f, ["xc", "identb"], ["pt0"])
        A("dve", lambda e: e.tensor_copy(out=Btok[:, :, :], in_=pt[0][:, 0:256].rearrange("p (g n) -> p g n", g=2)), ["pt0"], ["Btok"])
        A("dve", lambda e: e.tensor_tensor(out=v3(xdt[:, :], 16), in0=v3(xstok[:, :], 16), in1=bc(dtv[:, :].unsqueeze(2), [128, 16, 64]), op=ALU.mult), ["xstok", "dtv"], ["xdt"])
        A("dve", lambda e: e.tensor_tensor(out=v3(xdtw[:, :], 16), in0=v3(xdt[:, :], 16), in1=bc(te[:, :].unsqueeze(2), [128, 16, 64]), op=ALU.mult), ["xdt", "te"], ["xdtw"])
        if own:
            for g in range(2):
                p, ptok = nextp()
                A("pe", lambda e, p=p, g=g: e.matmul(p[:, 0:128], lhsT=xc[:, 8 + g, :], rhs=xc[:, 10 + g, :], start=True, stop=True), ["xc"], [ptok])
                A("act", lambda e, p=p, g=g: e.copy(out=cbt[:, g, :], in_=p[:, 0:128]), [ptok], ["cbt"])
            pyd = [(pg[4], "pg4"), (pg[5], "pg5")]
            for h in range(16):
                g = h // 8
                A("dve", lambda e, h=h: e.tensor_copy(out=abc[:, :], in_=bc(aa[:, h:h + 1], [128, 128])), ["aa"], ["abc"])
                p, ptok = nextp()
                def f(e, p=p):
                    e.matmul(p[:, 0:128], lhsT=abc[:, :], rhs=U[:, :], start=True, stop=False)
                    return e.matmul(p[:, 0:128], lhsT=identf[:, :], rhs=negm[:, :], start=False, stop=True)
                A("pe", f, ["abc", "U", "identf", "negm"], [ptok])
                A("act", lambda e, p=p, h=h: e.activation(out=dec[:, :], in_=p[:, 0:128], func=AF.Exp, bias=nacs[:, h:h + 1]), [ptok, "nacs"], ["dec"])
                A("pool", lambda e, g=g: e.tensor_tensor(out=Mb[:, :], in0=dec[:, :], in1=cbt[:, g, :], op=ALU.mult), ["dec", "cbt"], ["Mb"])
                A("pe", lambda e, h=h, g=g: e.matmul(pyd[g][0][:, (h % 8) * 64:(h % 8 + 1) * 64], lhsT=Mb[:, :], rhs=xdt[:, h * 64:(h + 1) * 64], start=True, stop=True), ["Mb", "xdt"], [pyd[g][1]])
            for g in range(2):
                gsl = slice(g * 512, (g + 1) * 512)
                po, potok = nextp()
                A("pe", lambda e, po=po, g=g, gsl=gsl: e.matmul(po[:, :], lhsT=xc[:, 10 + g, :], rhs=Hb[:, gsl], start=True, stop=True), ["xc", "Hb"], [potok])
                A("dve", lambda e, po=po, g=g, gsl=gsl: e.tensor_tensor(out=v3(ty[:, gsl], 8), in0=v3(po[:, :], 8), in1=bc(el[:, g * 8:(g + 1) * 8].unsqueeze(2), [128, 8, 64]), op=ALU.mult), [potok, "el"], ["ty"])
                A("dve", lambda e, g=g, gsl=gsl: e.tensor_tensor(out=ty[:, gsl], in0=ty[:, gsl], in1=pyd[g][0][:, :], op=ALU.add), ["ty", pyd[g][1]], ["ty"])
                A("pool", lambda e, g=g, gsl=gsl: e.tensor_tensor(out=v3(tu[:, :], 8), in0=v3(xstok[:, gsl], 8), in1=bc(dsk[:, g * 8:(g + 1) * 8].unsqueeze(2), [128, 8, 64]), op=ALU.mult), ["xstok", "dsk"], ["tu"])
                A("pool", lambda e, gsl=gsl: e.tensor_tensor(out=ty[:, gsl], in0=ty[:, gsl], in1=tu[:, :], op=ALU.add), ["ty", "tu"], ["ty"])
                A("pool", lambda e, gsl=gsl: e.tensor_tensor(out=ty[:, gsl], in0=ty[:, gsl], in1=zs[:, gsl], op=ALU.mult), ["ty", "zs"], ["ty"])
                A("act", lambda e, gsl=gsl: e.activation(out=junk[:, 0:512], in_=ty[:, gsl], func=AF.Square, accum_out=ss2[:, 0:1]), ["ty"], ["ss2", "junk"])
                A("act", lambda e: e.activation(out=r2[:, :], in_=ss2[:, :], func=AF.Sqrt, scale=1.0 / 512, bias=EPS), ["ss2"], ["r2"])
                A("dve", lambda e: e.reciprocal(out=r2[:, :], in_=r2[:, :]), ["r2"], ["r2"])
                A("dve", lambda e, g=g, gsl=gsl: e.scalar_tensor_tensor(out=ycat[:, 1024 + g * 512:1024 + (g + 1) * 512], in0=ty[:, gsl], scalar=r2[:, 0:1], in1=snw[:, gsl], op0=ALU.mult, op1=ALU.mult), ["ty", "r2", "snw"], ["ycat"])
        for g in range(2):
            gsl = slice(g * 512, (g + 1) * 512)
            pn, pntok = nextp()
            A("pe", lambda e, pn=pn, g=g, gsl=gsl: e.matmul(pn[:, :], lhsT=Btok[:, g, :], rhs=xdtw[:, gsl], start=True, stop=True), ["Btok", "xdtw"], [pntok])
            A("dve", lambda e, g=g, gsl=gsl: e.tensor_tensor(out=v3(H[:, gsl], 8), in0=v3(H[:, gsl], 8), in1=bc(cdec[:, g * 8:(g + 1) * 8].unsqueeze(2), [128, 8, 64]), op=ALU.mult), ["H", "cdec"], ["H"])
            A("dve", lambda e, pn=pn, gsl=gsl: e.tensor_tensor(out=H[:, gsl], in0=H[:, gsl], in1=pn[:, :], op=ALU.add), ["H", pntok], ["H"])
            A("pool", lambda e, gsl=gsl: e.tensor_copy(out=Hb[:, gsl], in_=H[:, gsl]), ["H"], ["Hb"])
        if upto <= 5:
            break
        if not own:
            continue

        DUMP("ycat", ycat[:, :], "ycat", 2048)
        DUMP("ty", ty[:, :], "ty", 1024)
        A("dve", lambda e: e.tensor_copy(out=ycb[:, :], in_=ycat[:, :]), ["ycat"], ["xn"])
        transposes16(ycb, "xn", yT, "yT")
        for nb in range(4):
            nsl = slice(nb * 512, (nb + 1) * 512)
            wb, wtok = wload(wout_d[:, nsl].rearrange("(k p) n -> p k n", p=128), lambda b: b[:, :].rearrange("p (k n) -> p k n", k=16))
            w3 = wb[:, :].rearrange("p (k n) -> p k n", k=16)
            p, ptok = nextp()
            def f(e, p=p, w3=w3):
                for k in range(16):
                    i = e.matmul(p[:, :], lhsT=yT[:, k, :], rhs=w3[:, k, :], start=(k == 0), stop=(k == 15))
                return i
            A("pe", f, [wtok, "yT"], [ptok])
            A("dve", lambda e, p=p, nsl=nsl: e.tensor_tensor(out=hbuf[:, nsl], in0=hbuf[:, nsl], in1=p[:, :], op=ALU.add), ["h", ptok], ["h"])
        DUMP("h1", hbuf[:, :], "h", 2048)
        if upto <= 6:
            break
        rms(hbuf, "h", g2, "g2", hn, "ycat")
        A("dve", lambda e: e.tensor_copy(out=xn[:, :], in_=hn[:, :]), ["ycat"], ["xn"])
        A("dve", lambda e: e.tensor_tensor(out=lob[:, :], in0=hn[:, :], in1=xn[:, :], op=ALU.subtract), ["ycat", "xn"], ["lob"])
        transposes16(xn, "xn", hnT, "hnT")
        transposes16(lob, "lob", loT, "loT")
        if upto <= 6.2:
            break
        p, ptok = nextp()
        def f(e, p=p):
            n = 0
            for (aT, w) in ((hnT, wrhi), (loT, wrhi), (hnT, wrlo)):
                for k in range(16):
                    i = e.matmul(p[:, 0:36], lhsT=aT[:, k, :], rhs=w[:, k, :], start=(n == 0), stop=(n == 47))
                    n += 1
            return i
        A("pe", f, ["hnT", "loT", "wrhi", "wrlo"], [ptok])
        A("dve", lambda e, p=p: e.tensor_tensor(out=lg[:, :], in0=p[:, 0:36], in1=rb[:, :], op=ALU.add), [ptok, "rb"], ["lg"])
        X = mybir.AxisListType.X
        if upto <= 6.4:
            break
        A("dve", lambda e: e.reduce_max(out=mx[:, :], in_=lg[:, 0:4], axis=X), ["lg"], ["mx"])
        A("dve", lambda e: e.tensor_scalar(out=ohg[:, :], in0=lg[:, 0:4], scalar1=mx[:, 0:1], scalar2=None, op0=ALU.is_equal), ["lg", "mx"], ["ohg"])
        A("dve", lambda e: e.tensor_scalar(out=nmx[:, :], in0=mx[:, :], scalar1=-1.0, scalar2=None, op0=ALU.mult), ["mx"], ["nmx"])
        A("act", lambda e: e.activation(out=eg[:, :], in_=lg[:, 0:4], func=AF.Exp, bias=nmx[:, 0:1], accum_out=sg[:, 0:1]), ["lg", "nmx"], ["eg", "sg"])
        A("dve", lambda e: e.reciprocal(out=psel[:, :], in_=sg[:, :]), ["sg"], ["psel"])
        if upto <= 6.6:
            break
        A("dve", lambda e: e.tensor_scalar(out=les[:, :], in0=lg[:, 4:12], scalar1=ohg[:, 0:1], scalar2=None, op0=ALU.mult), ["lg", "ohg"], ["les"])
        for g in (1, 2, 3):
            A("dve", lambda e, g=g: e.scalar_tensor_tensor(out=les[:, :], in0=lg[:, 4 + 8 * g:12 + 8 * g], scalar=ohg[:, g:g + 1], in1=les[:, :], op0=ALU.mult, op1=ALU.add), ["lg", "ohg", "les"], ["les"])
        A("dve", lambda e: e.reduce_max(out=m1[:, :], in_=les[:, :], axis=X), ["les"], ["m1"])
        A("dve", lambda e: e.tensor_scalar(out=k1[:, :], in0=les[:, :], scalar1=m1[:, 0:1], scalar2=None, op0=ALU.is_equal), ["les", "m1"], ["k1"])
        A("dve", lambda e: e.scalar_tensor_tensor(out=les2[:, :], in0=k1[:, :], scalar=-1e30, in1=les[:, :], op0=ALU.mult, op1=ALU.add), ["k1", "les"], ["les2"])
        A("dve", lambda e: e.reduce_max(out=m2[:, :], in_=les2[:, :], axis=X), ["les2"], ["m2"])
        A("dve", lambda e: e.tensor_scalar(out=k2[:, :], in0=les2[:, :], scalar1=m2[:, 0:1], scalar2=None, op0=ALU.is_equal), ["les2", "m2"], ["k2"])
        A("dve", lambda e: e.tensor_tensor(out=dd[:, :], in0=m2[:, :], in1=m1[:, :], op=ALU.subtract), ["m1", "m2"], ["dd"])
        A("act", lambda e: e.activation(out=ed[:, :], in_=dd[:, :], func=AF.Exp), ["dd"], ["ed"])
        A("dve", lambda e: e.tensor_scalar(out=w1[:, :], in0=ed[:, :], scalar1=1.0, scalar2=None, op0=ALU.add), ["ed"], ["w1"])
        A("dve", lambda e: e.reciprocal(out=w1[:, :], in_=w1[:, :]), ["w1"], ["w1"])
        A("dve", lambda e: e.tensor_tensor(out=w2[:, :], in0=ed[:, :], in1=w1[:, :], op=ALU.mult), ["ed", "w1"], ["w2"])
        A("dve", lambda e: e.tensor_scalar(out=wig[:, :], in0=k1[:, :], scalar1=w1[:, 0:1], scalar2=None, op0=ALU.mult), ["k1", "w1"], ["wig"])
        A("dve", lambda e: e.scalar_tensor_tensor(out=wig[:, :], in0=k2[:, :], scalar=w2[:, 0:1], in1=wig[:, :], op0=ALU.mult, op1=ALU.add), ["k2", "w2", "wig"], ["wig"])
        for g in range(4):
            A("dve", lambda e, g=g: e.tensor_tensor(out=scg[:, :], in0=ohg[:, g:g + 1], in1=psel[:, :], op=ALU.mult), ["ohg", "psel"], ["scg"])
            A("dve", lambda e, g=g: e.tensor_scalar(out=comb[:, g * 8:(g + 1) * 8], in0=wig[:, :], scalar1=scg[:, 0:1], scalar2=None, op0=ALU.mult), ["wig", "scg"], ["comb"])
        DUMP("comb", comb[:, :], "comb", 32)
        DUMP("lg", lg[:, :], "lg", 36)
        if upto <= 7:
            break
        for ex in range(32):
            kview = lambda b: b[:, :].rearrange("p (k n) -> p k n", k=16)
            wgb, wgtok = wload(wg_d[ex].rearrange("(k p) n -> p k n", p=128), kview)
            wub, wutok = wload(wu_d[ex].rearrange("(k p) n -> p k n", p=128), kview)
            pgt, pgtok = nextp()
            put, putok = nextp()
            def f(e, pp=pgt, w3=kview(wgb)):
                for k in range(16):
                    i = e.matmul(pp[:, :], lhsT=hnT[:, k, :], rhs=w3[:, k, :], start=(k == 0), stop=(k == 15))
                return i
            A("pe", f, [wgtok, "hnT"], [pgtok])
            def f(e, pp=put, w3=kview(wub)):
                for k in range(16):
                    i = e.matmul(pp[:, :], lhsT=hnT[:, k, :], rhs=w3[:, k, :], start=(k == 0), stop=(k == 15))
                return i
            A("pe", f, [wutok, "hnT"], [putok])
            wdb, wdtok = wload(wd_d[ex].rearrange("(k p) n -> p k n", p=128), lambda b: b[:, :].rearrange("p (k n) -> p k n", k=4))
            A("act", lambda e, pp=pgt: e.activation(out=sgl[:, :], in_=pp[:, :], func=AF.Silu), [pgtok], ["sgl"])
            A("dve", lambda e, pp=put, ex=ex: e.scalar_tensor_tensor(out=actb[:, :], in0=sgl[:, :], scalar=comb[:, ex:ex + 1], in1=pp[:, :], op0=ALU.mult, op1=ALU.mult), ["sgl", "comb", putok], ["actb"])
            def f(e):
                for k in range(4):
                    i = e.transpose(out=pt[0][:, k * 128:(k + 1) * 128], in_=actb[:, k * 128:(k + 1) * 128], identity=identb[:, :])
                return i
            A("pe", f, ["actb", "identb"], ["pt0"])
            A("act", lambda e: e.copy(out=actT[:, :, :], in_=pt[0][:, 0:512].rearrange("p (k t) -> p k t", k=4)), ["pt0"], ["actT"])
            wd3 = wdb[:, :].rearrange("p (k n) -> p k n", k=4)
            for nb in range(4):
                nsl = slice(nb * 512, (nb + 1) * 512)
                pd, pdtok = nextp()
                def f(e, pd=pd, wd3=wd3, nsl=nsl):
                    for k in range(4):
                        i = e.matmul(pd[:, :], lhsT=actT[:, k, :], rhs=wd3[:, k, nsl], start=(k == 0), stop=(k == 3))
                    return i
                A("pe", f, [wdtok, "actT"], [pdtok])
                A("dve", lambda e, pd=pd, nsl=nsl: e.tensor_tensor(out=hbuf[:, nsl], in0=hbuf[:, nsl], in1=pd[:, :], op=ALU.add), ["h", pdtok], ["h"])
        DUMP("h2", hbuf[:, :], "h", 2048)
        rms(hbuf, "h", gf, "gf", obuf, "ycat")
        oc = c - OWN0
        A("sp", lambda e, oc=oc: e.dma_start(out=out_d[oc * 128:(oc + 1) * 128, :], in_=obuf[:, :]), ["ycat"], ["outd"], dma="out")

    s.emit()
    stack.close()
    return nc


_NC_CACHE = {}


def kernel(x, positions, norm1_w, w_in, conv_w, conv_b, dt_bias, a_log, d_skip, ret_norm_w,
           ssm_norm_w, w_out, norm2_w, w_router_group, b_router_group, w_router_expert,
           b_router_expert, w_expert_gate, w_expert_up, w_expert_down, final_norm_w):
    f32 = np.float32
    x2 = np.asarray(x, f32).reshape(SEQ, D)
    pos = np.asarray(positions, np.int32).reshape(1, SEQ)
    idx = np.arange(128, dtype=np.float64)
    gam = 1.0 - 2.0 ** (-5.0 - np.arange(4, dtype=np.float64))
    rqv = (gam[:, None] ** (idx[None, :] + 1)).astype(f32).reshape(1, 512)
    rkv = ((gam[:, None] ** (-(idx[None, :] + 1))) * (256 ** -0.5)).astype(f32).reshape(1, 512)
    Gv = (gam ** 128).astype(f32).reshape(1, 4)
    invf = (10000.0 ** (-np.arange(128, dtype=f32) / 128)).astype(f32).reshape(128, 1)
    ident = np.eye(128, dtype=f32)
    si, li = np.meshgrid(np.arange(128), np.arange(128), indexing="ij")
    caus = (li >= si).astype(f32)
    Um = (si <= li).astype(f32)
    negm = np.where(li >= si, 0.0, -30000.0).astype(f32)
    shared = {
        "norm1_w": np.asarray(norm1_w, f32).reshape(1, D),
        "w_in": np.asarray(w_in, f32).reshape(D, INP),
        "conv_w": np.ascontiguousarray(np.asarray(conv_w, f32).reshape(4, 12, 128).transpose(2, 1, 0)),
        "conv_b": np.ascontiguousarray(np.asarray(conv_b, f32).reshape(12, 128).T),
        "dt_bias": np.asarray(dt_bias, f32).reshape(1, 16),
        "a_log": np.asarray(a_log, f32).reshape(1, 16),
        "d_skip": np.asarray(d_skip, f32).reshape(1, 16),
        "ret_norm_w": np.asarray(ret_norm_w, f32).reshape(1, 1024),
        "ssm_norm_w": np.asarray(ssm_norm_w, f32).reshape(1, 1024),
        "w_out": np.asarray(w_out, f32).reshape(D, D),
        "norm2_w": np.asarray(norm2_w, f32).reshape(1, D),
        "w_router": np.ascontiguousarray(np.concatenate([np.asarray(w_router_group, f32).reshape(D, 4), np.asarray(w_router_expert, f32).reshape(D, 32)], axis=1)),
        "b_router": np.concatenate([np.asarray(b_router_group, f32).reshape(1, 4), np.asarray(b_router_expert, f32).reshape(1, 32)], axis=1),
        "w_gate": np.asarray(w_expert_gate, f32).reshape(32, D, 512),
        "w_up": np.asarray(w_expert_up, f32).reshape(32, D, 512),
        "w_down": np.asarray(w_expert_down, f32).reshape(32, 512, D),
        "final_norm_w": np.asarray(final_norm_w, f32).reshape(1, D),
        "ident": ident, "caus": caus, "U": Um, "negm": negm, "rq": rqv, "rk": rkv, "G": Gv, "inv": invf,
    }
    in_maps = []
    for i in range(NCORE):
        n_real = TOK * (i + 1)
        xe = np.zeros((SEQ, D), f32)
        xe[SEQ - n_real:] = x2[:n_real]
        pe_ = np.zeros((1, SEQ), np.int32)
        pe_[0, SEQ - n_real:] = pos[0, :n_real]
        val = np.zeros(SEQ, f32)
        val[SEQ - n_real:] = 1.0
        m = dict(shared)
        m["x"] = xe
        m["pos"] = pe_
        m["valid"] = np.ascontiguousarray(val.reshape(NCH, 128).T)
        in_maps.append(m)
    if "nc" not in _NC_CACHE:
        _NC_CACHE["nc"] = build_nc()
    res = run_bass_kernel_spmd(_NC_CACHE["nc"], in_maps, core_ids=list(range(NCORE)))
    out = np.concatenate([np.asarray(r["out"], f32) for r in res.results], axis=0)
    return out.reshape(1, SEQ, D)
```

```python
import contextlib
import numpy as np
import concourse.bass as bass
import concourse.mybir as mybir
from concourse.bass_utils import run_bass_kernel_spmd

F32 = mybir.dt.float32
BF16 = mybir.dt.bfloat16
I32 = mybir.dt.int32
AF = mybir.ActivationFunctionType
ALU = mybir.AluOpType
ENGS = ("pe", "act", "dve", "pool", "sp")
NCORE = 8
D = 2048
SEQ = 8192
TOK = SEQ // NCORE
NCH = SEQ // 128
OWN0 = NCH - TOK // 128
INP = 6672
EPS = 1e-6


class Tok:
    __slots__ = ("name", "writer", "readers")

    def __init__(self, name):
        self.name = name
        self.writer = None
        self.readers = []


class Op:
    __slots__ = ("eng", "fn", "deps", "dma_sem", "dma_val", "signal", "sig_val")


class Sched:
    def __init__(self, nc):
        self.nc = nc
        self.ops = {e: [] for e in ENGS}
        self.dma_counts = {}
        self.tk = {}

    def T(self, name):
        t = self.tk.get(name)
        if t is None:
            t = self.tk[name] = Tok(name)
        return t

    def add(self, eng, fn, reads=(), writes=(), dma=None):
        op = Op()
        op.eng, op.fn, op.deps, op.signal, op.sig_val = eng, fn, [], False, None
        op.dma_sem, op.dma_val = dma, None
        if dma is not None:
            c = self.dma_counts.get(dma, 0) + 16
            self.dma_counts[dma] = c
            op.dma_val = c
        deps = {}
        rt = [self.T(x) for x in reads]
        wt = [self.T(x) for x in writes]
        for t in rt:
            if t.writer is not None:
                deps[id(t.writer)] = t.writer
        for t in wt:
            if t.writer is not None:
                deps[id(t.writer)] = t.writer
            for r in t.readers:
                deps[id(r)] = r
        for d in deps.values():
            if d.dma_sem is None and d.eng == "pe" and eng == "pe" and dma is None:
                continue
            op.deps.append(d)
            if d.dma_sem is None:
                d.signal = True
        for t in rt:
            t.readers.append(op)
        for t in wt:
            t.writer = op
            t.readers = []
        self.ops[eng].append(op)
        return op

    def group_close(self, dma):
        tot = self.dma_counts[dma]
        for e in ENGS:
            for op in self.ops[e]:
                if op.dma_sem == dma:
                    op.dma_val = tot

    def emit(self):
        nc = self.nc
        with contextlib.ExitStack() as stack:
            block = stack.enter_context(nc.Block())
            esem = {e: stack.enter_context(nc.semaphore(f"s_{e}")) for e in ENGS if e != "sp"}
            dsem = {k: stack.enter_context(nc.semaphore(f"d_{k}")) for k in self.dma_counts}
            for e in ENGS:
                c = 0
                for op in self.ops[e]:
                    if op.dma_sem is None and op.signal:
                        c += 1
                        op.sig_val = c
            ops = self.ops
            counts = self.dma_counts

            def run(e, engobj):
                waited = {}
                for op in ops[e]:
                    for d in op.deps:
                        if d.dma_sem is not None:
                            key, sem, val = ("d", d.dma_sem), dsem[d.dma_sem], d.dma_val
                        else:
                            key, sem, val = ("e", d.eng), esem[d.eng], d.sig_val
                        if waited.get(key, 0) >= val:
                            continue
                        waited[key] = val
                        engobj.wait_ge(sem, val)
                    ins = op.fn(engobj)
                    if op.dma_sem is not None:
                        ins.then_inc(dsem[op.dma_sem], 16)
                    elif op.signal:
                        ins.then_inc(esem[e], 1)
                if e == "sp":
                    for k, c in counts.items():
                        engobj.wait_ge(dsem[k], c)

            @block.tensor
            def _(pe):
                run("pe", pe)

            @block.scalar
            def _(act):
                run("act", act)

            @block.vector
            def _(dve):
                run("dve", dve)

            @block.gpsimd
            def _(pool):
                run("pool", pool)

            @block.sync
            def _(sp):
                run("sp", sp)


def build_nc(chunks=None, upto=99, dump=None):
    nc = bass.Bass("TRN2", target_bir_lowering=False)
    DI = lambda n, sh, dt=F32: nc.dram_tensor(n, sh, dt, kind="ExternalInput").ap()
    x_d = DI("x", [SEQ, D])
    pos_d = DI("pos", [1, SEQ], I32)
    valid_d = DI("valid", [128, NCH])
    g1_d = DI("norm1_w", [1, D])
    win_d = DI("w_in", [D, INP])
    cw_d = DI("conv_w", [128, 12, 4])
    cb_d = DI("conv_b", [128, 12])
    dtb_d = DI("dt_bias", [1, 16])
    alog_d = DI("a_log", [1, 16])
    dsk_d = DI("d_skip", [1, 16])
    rnw_d = DI("ret_norm_w", [1, 1024])
    snw_d = DI("ssm_norm_w", [1, 1024])
    wout_d = DI("w_out", [D, D])
    g2_d = DI("norm2_w", [1, D])
    wr_d = DI("w_router", [D, 36])
    rb_d = DI("b_router", [1, 36])
    wg_d = DI("w_gate", [32, D, 512])
    wu_d = DI("w_up", [32, D, 512])
    wd_d = DI("w_down", [32, 512, D])
    gf_d = DI("final_norm_w", [1, D])
    ident_d = DI("ident", [128, 128])
    caus_d = DI("caus", [128, 128])
    U_d = DI("U", [128, 128])
    negm_d = DI("negm", [128, 128])
    rq_d = DI("rq", [1, 512])
    rk_d = DI("rk", [1, 512])
    G_d = DI("G", [1, 4])
    inv_d = DI("inv", [128, 1])
    out_d = nc.dram_tensor("out", [TOK, D], F32, kind="ExternalOutput").ap()
    dbg_d = nc.dram_tensor("dbg", [128, D], F32, kind="ExternalOutput").ap() if dump else None

    s = Sched(nc)
    stackP = contextlib.ExitStack()
    stackM = contextlib.ExitStack()
    SBP = lambda n, sh, dt=F32: stackP.enter_context(nc.sbuf_tensor("sb_" + n, sh, dt))
    PI = float(np.pi)
    NW = 2
    wts = [SBP(f"wt{i}", [128, 16 * 512], BF16) for i in range(NW)]
    yTall = SBP("yTall", [128, 16, TOK], BF16)
    identb = SBP("identb", [128, 128], BF16)
    rb = SBP("rb", [128, 36]); wrhi = SBP("wrhi", [128, 16, 36], BF16); wrlo = SBP("wrlo", [128, 16, 36], BF16)
    ss = SBP("ss", [128, 1]); rstd = SBP("rstd", [128, 1])
    lg = SBP("lg", [128, 36]); mx = SBP("mx", [128, 1]); nmx = SBP("nmx", [128, 1]); ohg = SBP("ohg", [128, 4])
    eg = SBP("eg", [128, 4]); sg = SBP("sg", [128, 1]); psel = SBP("psel", [128, 1])
    les = SBP("les", [128, 8]); les2 = SBP("les2", [128, 8]); k1 = SBP("k1", [128, 8]); k2 = SBP("k2", [128, 8])
    m1 = SBP("m1", [128, 1]); m2 = SBP("m2", [128, 1]); dd = SBP("dd", [128, 1]); ed = SBP("ed", [128, 1]); w1 = SBP("w1", [128, 1]); w2 = SBP("w2", [128, 1])
    wig = SBP("wig", [128, 8]); scg = SBP("scg", [128, 1])
    bars = {e: SBP(f"bar_{e}", [128, 2]) for e in ("act", "dve", "pool")}
    pg = [stackP.enter_context(nc.psum_tensor(f"pg{i}", [128, 512], F32)) for i in range(6)]
    pt = [stackP.enter_context(nc.psum_tensor(f"pt{i}", [128, 1024], BF16)) for i in range(2)]
    SB = lambda n, sh, dt=F32: stackM.enter_context(nc.sbuf_tensor("sb_" + n, sh, dt))

    xin = SB("xin", [128, D]); xn = SB("xn", [128, D], BF16)
    g1 = SB("g1", [128, D])
    rnw = SB("rnw", [128, 1024]); snw = SB("snw", [128, 1024])
    xnT = SB("xnT", [128, 16, 128], BF16)
    identf = SB("identf", [128, 128])
    causb = SB("causb", [128, 128], BF16); causf = SB("causf", [128, 128])
    U = SB("U", [128, 128]); negm = SB("negm", [128, 128]); ones = SB("ones", [128, 128])
    rq = SB("rq", [128, 4, 128]); rk = SB("rk", [128, 4, 128]); Gb = SB("Gb", [128, 4])
    inv = SB("inv", [128, 1]); valid = SB("valid", [128, NCH])
    cw = SB("cw", [128, 12, 4]); cb = SB("cb", [128, 12])
    dtb = SB("dtb", [128, 16]); negA = SB("negA", [128, 16]); dsk = SB("dsk", [128, 16])
    wr = SB("wr", [128, 16, 36])
    posi = SB("posi", [128, 128], I32); pf = SB("pf", [128, 128]); ang = SB("ang", [128, 128])
    ra = SB("ra", [128, 128]); ki = SB("ki", [128, 128], I32); kf = SB("kf", [128, 128]); mk = SB("mk", [128, 128])
    cos = SB("cos", [128, 128]); sin = SB("sin", [128, 128])
    rt = [SB(f"rt{i}", [128, 128]) for i in range(6)]
    qT = SB("qT", [128, 8, 128], BF16); kT = SB("kT", [128, 8, 128], BF16)
    v = SB("v", [128, 1024], BF16); gs = SB("gs", [128, 1024]); zs = SB("zs", [128, 1024])
    xbc = SB("xbc", [128, 12, 131]); xc = SB("xc", [128, 12, 128], BF16)
    cvo = [SB(f"cvo{i}", [128, 128]) for i in range(2)]
    dtr = SB("dtr", [128, 16]); dt = SB("dt", [128, 16]); dtv = SB("dtv", [128, 16]); aa = SB("aa", [128, 16])
    acs = SB("acs", [128, 16]); nacs = SB("nacs", [128, 16]); cdec = SB("cdec", [128, 16]); te = SB("te", [128, 16]); el = SB("el", [128, 16])
    tmp16 = SB("tmp16", [128, 16])
    ktok = SB("ktok", [128, 1024], BF16); xstok = SB("xstok", [128, 1024], BF16); Btok = SB("Btok", [128, 2, 128], BF16)
    scm = SB("scm", [128, 128], BF16)
    st6 = SB("st6", [128, 6]); mv = SB("mv", [128, 2]); rs = SB("rs", [128, 1])
    yn = SB("yn", [128, 256]); ycat = SB("ycat", [128, D]); ycb = xn; junk = xn
    S = SB("S", [128, 4, 512]); Sb = SB("Sb", [128, 4, 512], BF16)
    H = SB("H", [128, 1024]); Hb = SB("Hb", [128, 1024], BF16)
    xdt = SB("xdt", [128, 1024], BF16); xdtw = SB("xdtw", [128, 1024], BF16)
    cbt = SB("cbt", [128, 2, 128]); abc = SB("abc", [128, 128]); dec = SB("dec", [128, 128]); Mb = SB("Mb", [128, 128], BF16)
    ty = SB("ty", [128, 1024]); tu = SB("tu", [128, 512]); ss2 = SB("ss2", [128, 1]); r2 = SB("r2", [128, 1])
    pcnt = [0]

    def nextp():
        i = pcnt[0] % 4
        pcnt[0] += 1
        return pg[i], f"pg{i}"

    wcnt = [0]

    def wload(src_ap, view):
        i = wcnt[0] % NW
        wcnt[0] += 1
        buf = wts[i]
        s.add("pool", lambda e: e.dma_start(out=view(buf), in_=src_ap), writes=[f"wt{i}"], dma=f"w{i}")
        return buf, f"wt{i}"

    A = s.add

    def DUMP(name, ap, tok, width):
        if dump == name:
            A("pool", lambda e: e.dma_start(out=dbg_d[:, 0:width], in_=ap), [tok], ["dbgd"], dma="dbg")
    bc = lambda ap, sh: ap.to_broadcast(sh)

    def cload(buf_ap, src, tok):
        A("sp", lambda e: e.dma_start(out=buf_ap, in_=src), writes=[tok], dma="const")
    cload(g1[:, :], g1_d[0:1, :].partition_broadcast(128), "g1")
    cload(rnw[:, :], rnw_d[0:1, :].partition_broadcast(128), "rnw")
    cload(snw[:, :], snw_d[0:1, :].partition_broadcast(128), "snw")
    cload(identf[:, :], ident_d[:, :], "identf")
    cload(causf[:, :], caus_d[:, :], "causf")
    cload(U[:, :], U_d[:, :], "U")
    cload(negm[:, :], negm_d[:, :], "negm")
    cload(rq[:, :, :].rearrange("p h l -> p (h l)"), rq_d[0:1, :].partition_broadcast(128), "rq")
    cload(rk[:, :, :].rearrange("p h l -> p (h l)"), rk_d[0:1, :].partition_broadcast(128), "rk")
    cload(Gb[:, :], G_d[0:1, :].partition_broadcast(128), "Gb")
    cload(inv[:, :], inv_d[:, :], "inv")
    cload(valid[:, :], valid_d[:, :], "valid")
    cload(cw[:, :, :], cw_d[:, :, :], "cw")
    cload(cb[:, :], cb_d[:, :], "cb")
    cload(dtb[:, :], dtb_d[0:1, :].partition_broadcast(128), "dtb")
    cload(negA[:, :], alog_d[0:1, :].partition_broadcast(128), "negA")
    cload(dsk[:, :], dsk_d[0:1, :].partition_broadcast(128), "dsk")
    cload(wr[:, :, :], wr_d.rearrange("(k p) n -> p k n", p=128), "wr")
    cload(rb[:, :], rb_d[0:1, :].partition_broadcast(128), "rb")
    s.group_close("const")
    A("dve", lambda e: e.tensor_copy(out=identb[:, :], in_=identf[:, :]), ["identf"], ["identb"])
    A("dve", lambda e: e.tensor_copy(out=causb[:, :], in_=causf[:, :]), ["causf"], ["causb"])
    A("dve", lambda e: e.memset(ones[:, :], 1.0), [], ["ones"])
    A("dve", lambda e: e.tensor_copy(out=wrhi[:, :, :], in_=wr[:, :, :]), ["wr"], ["wrhi"])
    A("dve", lambda e: e.tensor_tensor(out=wrlo[:, :, :], in0=wr[:, :, :], in1=wrhi[:, :, :], op=ALU.subtract), ["wr", "wrhi"], ["wrlo"])
    A("act", lambda e: e.activation(out=negA[:, :], in_=negA[:, :], func=AF.Exp), ["negA"], ["negA"])
    A("dve", lambda e: e.tensor_scalar(out=negA[:, :], in0=negA[:, :], scalar1=-1.0, scalar2=None, op0=ALU.mult), ["negA"], ["negA"])
    A("dve", lambda e: e.memset(xbc[:, :, :], 0.0), [], ["xbc"])
    A("dve", lambda e: e.memset(S[:, :, :], 0.0), [], ["S"])
    A("dve", lambda e: e.memset(Sb[:, :, :], 0.0), [], ["Sb"])
    A("dve", lambda e: e.memset(H[:, :], 0.0), [], ["H"])
    A("dve", lambda e: e.memset(Hb[:, :], 0.0), [], ["Hb"])

    def rms(src, srctok, wbuf, wtok, dst, dsttok, jk, jktok):
        A("act", lambda e: e.activation(out=jk, in_=src, func=AF.Square, accum_out=ss[:, 0:1]), [srctok], ["ss", jktok])
        A("act", lambda e: e.activation(out=rstd[:, :], in_=ss[:, :], func=AF.Sqrt, scale=1.0 / D, bias=EPS), ["ss"], ["rstd"])
        A("dve", lambda e: e.reciprocal(out=rstd[:, :], in_=rstd[:, :]), ["rstd"], ["rstd"])
        A("dve", lambda e: e.scalar_tensor_tensor(out=dst, in0=src, scalar=rstd[:, 0:1], in1=wbuf[:, :], op0=ALU.mult, op1=ALU.mult), [srctok, "rstd", wtok], [dsttok])

    def transposes16(src, srctok, dstf, dsttok):
        for half in range(2):
            p = pt[half]
            def f(e, half=half, p=p):
                for k in range(8):
                    kk = half * 8 + k
                    i = e.transpose(out=p[:, k * 128:(k + 1) * 128], in_=src[:, kk * 128:(kk + 1) * 128], identity=identb[:, :])
                return i
            A("pe", f, [srctok, "identb"], [f"pt{half}"])
            if half == 0:
                A("act", lambda e, half=half, p=p: e.copy(out=dstf(half), in_=p[:, :].rearrange("p (k t) -> p k t", k=8)), [f"pt{half}"], [dsttok])
            else:
                A("dve", lambda e, half=half, p=p: e.tensor_copy(out=dstf(half), in_=p[:, :].rearrange("p (k t) -> p k t", k=8)), [f"pt{half}"], [dsttok])

    def sincos(shift, outbuf, outtok):
        A("dve", lambda e: e.tensor_scalar(out=ra[:, :], in0=ang[:, :], scalar1=float(shift), scalar2=None, op0=ALU.add), ["ang"], ["ra"])
        A("dve", lambda e: e.tensor_scalar(out=ki[:, :], in0=ra[:, :], scalar1=float(1 / (2 * PI)), scalar2=None, op0=ALU.mult), ["ra"], ["ki"])
        A("dve", lambda e: e.tensor_copy(out=kf[:, :], in_=ki[:, :]), ["ki"], ["kf"])
        A("dve", lambda e: e.scalar_tensor_tensor(out=ra[:, :], in0=kf[:, :], scalar=float(-2 * PI), in1=ra[:, :], op0=ALU.mult, op1=ALU.add), ["kf", "ra"], ["ra"])
        A("dve", lambda e: e.tensor_scalar(out=mk[:, :], in0=ra[:, :], scalar1=PI, scalar2=float(-2 * PI), op0=ALU.is_gt, op1=ALU.mult), ["ra"], ["mk"])
        A("dve", lambda e: e.tensor_tensor(out=ra[:, :], in0=ra[:, :], in1=mk[:, :], op=ALU.add), ["ra", "mk"], ["ra"])
        A("act", lambda e: e.activation(out=outbuf[:, :], in_=ra[:, :], func=AF.Sin), ["ra"], [outtok])

    v3 = lambda ap, h: ap.rearrange("p (h j) -> p h j", h=h)

    for c in (range(NCH) if chunks is None else chunks):
        own = c >= OWN0
        cs = slice(c * 128, (c + 1) * 128)
        A("sp", lambda e, cs=cs: e.dma_start(out=xin[:, :], in_=x_d[cs, :]), [], ["xin"], dma="x")
        A("sp", lambda e, cs=cs: e.dma_start(out=posi[:, :], in_=pos_d[0:1, cs].partition_broadcast(128)), [], ["posi"], dma="pos")
        rms(xin[:, :], "xin", g1, "g1", xn[:, :], "xn", junk[:, :], "xn")
        transposes16(xn, "xn", lambda half: xnT[:, half * 8:(half + 1) * 8, :], "xnT")
        A("dve", lambda e: e.tensor_copy(out=pf[:, :], in_=posi[:, :]), ["posi"], ["pf"])
        A("dve", lambda e: e.tensor_scalar(out=ang[:, :], in0=pf[:, :], scalar1=inv[:, 0:1], scalar2=None, op0=ALU.mult), ["pf", "inv"], ["ang"])
        sincos(0.0, sin, "sin")
        sincos(PI / 2, cos, "cos")

        if upto <= 1:
            break
        DUMP("xn", xn[:, :], "xn", 2048)
        DUMP("cos", cos[:, :], "cos", 128)
        DUMP("sin", sin[:, :], "sin", 128)
        tiles = list(range(14)) if own else [2, 3, 4, 5, 10, 11, 12, 13]
        for t in tiles:
            wcols = 512 if t < 13 else 16
            wb, wtok = wload(win_d[:, t * 512:t * 512 + wcols].rearrange("(k p) n -> p k n", p=128),
                             lambda b, wcols=wcols: b[:, :].rearrange("p (k n) -> p k n", k=16)[:, :, 0:wcols])
            w3 = wb[:, :].rearrange("p (k n) -> p k n", k=16)
            if t in (0, 1, 2, 3, 10, 11, 12):
                p, ptok = nextp()
                def f(e, p=p, w3=w3):
                    for b in range(4):
                        for k in range(16):
                            i = e.matmul(p[:, b * 128:(b + 1) * 128], lhsT=w3[:, k, b * 128:(b + 1) * 128], rhs=xnT[:, k, :], start=(k == 0), stop=(k == 15))
                    return i
                A("pe", f, [wtok, "xnT"], [ptok])
                if t >= 10:
                    A("act", lambda e, p=p, t=t: e.copy(out=xbc[:, (t - 10) * 4:(t - 10) * 4 + 4, 3:131], in_=p[:, :].rearrange("p (b t) -> p b t", b=4)), [ptok], ["xbc"])
                else:
                    isq = t < 2
                    dstT, dtok, tbl, tbtok = (qT, "qT", rq, "rq") if isq else (kT, "kT", rk, "rk")
                    for hh in range(2):
                        h = (t % 2) * 2 + hh
                        x1 = p[:, (2 * hh) * 128:(2 * hh + 1) * 128]
                        x2 = p[:, (2 * hh + 1) * 128:(2 * hh + 2) * 128]
                        A("dve", lambda e, x1=x1: e.tensor_tensor(out=rt[0][:, :], in0=x1, in1=cos[:, :], op=ALU.mult), [ptok, "cos"], ["rt0"])
                        A("dve", lambda e, x2=x2: e.tensor_tensor(out=rt[1][:, :], in0=x2, in1=sin[:, :], op=ALU.mult), [ptok, "sin"], ["rt1"])
                        A("dve", lambda e, x2=x2: e.tensor_tensor(out=rt[2][:, :], in0=x2, in1=cos[:, :], op=ALU.mult), [ptok, "cos"], ["rt2"])
                        A("dve", lambda e, x1=x1: e.tensor_tensor(out=rt[3][:, :], in0=x1, in1=sin[:, :], op=ALU.mult), [ptok, "sin"], ["rt3"])
                        A("pool", lambda e: e.tensor_tensor(out=rt[4][:, :], in0=rt[0][:, :], in1=rt[1][:, :], op=ALU.subtract), ["rt0", "rt1"], ["rt4"])
                        A("pool", lambda e: e.tensor_tensor(out=rt[5][:, :], in0=rt[2][:, :], in1=rt[3][:, :], op=ALU.add), ["rt2", "rt3"], ["rt5"])
                        A("pool", lambda e, h=h, dstT=dstT, tbl=tbl: e.tensor_tensor(out=dstT[:, 2 * h, :], in0=rt[4][:, :], in1=tbl[:, h, :], op=ALU.mult), ["rt4", tbtok], [dtok])
                        A("pool", lambda e, h=h, dstT=dstT, tbl=tbl: e.tensor_tensor(out=dstT[:, 2 * h + 1, :], in0=rt[5][:, :], in1=tbl[:, h, :], op=ALU.mult), ["rt5", tbtok], [dtok])
            else:
                p, ptok = nextp()
                def f(e, p=p, w3=w3, wcols=wcols):
                    for k in range(16):
                        i = e.matmul(p[:, 0:wcols], lhsT=xnT[:, k, :], rhs=w3[:, k, 0:wcols], start=(k == 0), stop=(k == 15))
                    return i
                A("pe", f, [wtok, "xnT"], [ptok])
                if t in (4, 5):
                    A("act", lambda e, p=p, t=t: e.copy(out=v[:, (t - 4) * 512:(t - 3) * 512], in_=p[:, :]), [ptok], ["v"])
                elif t in (6, 7):
                    A("act", lambda e, p=p, t=t: e.activation(out=gs[:, (t - 6) * 512:(t - 5) * 512], in_=p[:, :], func=AF.Silu), [ptok], ["gs"])
                elif t in (8, 9):
                    A("act", lambda e, p=p, t=t: e.activation(out=zs[:, (t - 8) * 512:(t - 7) * 512], in_=p[:, :], func=AF.Silu), [ptok], ["zs"])
                else:
                    A("dve", lambda e, p=p: e.tensor_tensor(out=dtr[:, :], in0=p[:, 0:16], in1=dtb[:, :], op=ALU.add), [ptok, "dtb"], ["dtr"])
                    A("act", lambda e: e.activation(out=dtr[:, :], in_=dtr[:, :], func=AF.Exp), ["dtr"], ["dtr"])
                    A("act", lambda e: e.activation(out=dt[:, :], in_=dtr[:, :], func=AF.Ln, bias=1.0), ["dtr"], ["dt"])
        if upto <= 2:
            break
        for blk in range(12):
            o = cvo[blk % 2]
            otok = f"cvo{blk % 2}"
            A("dve", lambda e, o=o, blk=blk: e.tensor_scalar(out=o[:, :], in0=xbc[:, blk, 0:128], scalar1=cw[:, blk, 0:1], scalar2=None, op0=ALU.mult), ["xbc", "cw"], [otok])
            for i in (1, 2, 3):
                A("dve", lambda e, o=o, blk=blk, i=i: e.scalar_tensor_tensor(out=o[:, :], in0=xbc[:, blk, i:i + 128], scalar=cw[:, blk, i:i + 1], in1=o[:, :], op0=ALU.mult, op1=ALU.add), ["xbc", "cw", otok], [otok])
            A("act", lambda e, o=o, blk=blk: e.activation(out=xc[:, blk, :], in_=o[:, :], func=AF.Silu, bias=cb[:, blk:blk + 1]), [otok, "cb"], ["xc"])
        A("pool", lambda e: e.tensor_copy(out=xbc[:, :, 0:3], in_=xbc[:, :, 128:131]), ["xbc"], ["xbc"])

        if upto <= 3:
            break
        DUMP("qT", qT[:, :, :].rearrange("p a b -> p (a b)"), "qT", 1024)
        DUMP("kT", kT[:, :, :].rearrange("p a b -> p (a b)"), "kT", 1024)
        DUMP("v", v[:, :], "v", 1024)
        DUMP("gs", gs[:, :], "gs", 1024)
        DUMP("xc", xc[:, :, :].rearrange("p a b -> p (a b)"), "xc", 1536)
        DUMP("dt", dt[:, :], "dt", 16)
        def f(e):
            for k in range(8):
                i = e.transpose(out=pt[0][:, k * 128:(k + 1) * 128], in_=kT[:, k, :], identity=identb[:, :])
            return i
        A("pe", f, ["kT", "identb"], ["pt0"])
        A("act", lambda e: e.copy(out=ktok[:, :], in_=pt[0][:, :]), ["pt0"], ["ktok"])
        for h in range(4):
            hs = slice(h * 256, (h + 1) * 256)
            if own:
                p, ptok = nextp()
                def f(e, p=p, h=h):
                    for hf in range(2):
                        i = e.matmul(p[:, 0:128], lhsT=kT[:, 2 * h + hf, :], rhs=qT[:, 2 * h + hf, :], start=(hf == 0), stop=(hf == 1))
                    return i
                A("pe", f, ["kT", "qT"], [ptok])
                A("dve", lambda e, p=p: e.tensor_tensor(out=scm[:, :], in0=p[:, 0:128], in1=causf[:, :], op=ALU.mult), [ptok, "causf"], ["scm"])
                py, pytok = nextp()
                def f(e, py=py, h=h, hs=hs):
                    e.matmul(py[:, 0:256], lhsT=scm[:, :], rhs=v[:, hs], start=True, stop=False)
                    for hf in range(2):
                        i = e.matmul(py[:, 0:256], lhsT=qT[:, 2 * h + hf, :], rhs=Sb[:, h, hf * 256:(hf + 1) * 256], start=False, stop=(hf == 1))
                    return i
                A("pe", f, ["scm", "v", "qT", "Sb"], [pytok])
                A("dve", lambda e, py=py: e.bn_stats(out=st6[:, :], in_=py[:, 0:256]), [pytok], ["st6"])
                A("dve", lambda e: e.bn_aggr(out=mv[:, :], in_=st6[:, :]), ["st6"], ["mv"])
                A("act", lambda e: e.activation(out=rs[:, :], in_=mv[:, 1:2], func=AF.Sqrt, bias=EPS), ["mv"], ["rs"])
                A("dve", lambda e: e.reciprocal(out=rs[:, :], in_=rs[:, :]), ["rs"], ["rs"])
                A("dve", lambda e, py=py: e.tensor_scalar(out=yn[:, :], in0=py[:, 0:256], scalar1=mv[:, 0:1], scalar2=rs[:, 0:1], op0=ALU.subtract, op1=ALU.mult), [pytok, "mv", "rs"], ["yn"])
                A("pool", lambda e, hs=hs: e.tensor_tensor(out=yn[:, :], in0=yn[:, :], in1=rnw[:, hs], op=ALU.mult), ["yn", "rnw"], ["yn"])
                A("pool", lambda e, hs=hs: e.tensor_tensor(out=ycat[:, hs], in0=yn[:, :], in1=gs[:, hs], op=ALU.mult), ["yn", "gs"], ["ycat"])
            pk, pktok = nextp()
            def f(e, pk=pk, h=h, hs=hs):
                for hf in range(2):
                    i = e.matmul(pk[:, hf * 256:(hf + 1) * 256], lhsT=ktok[:, h * 256 + hf * 128:h * 256 + (hf + 1) * 128], rhs=v[:, hs], start=True, stop=True)
                return i
            A("pe", f, ["ktok", "v"], [pktok])
            A("dve", lambda e, pk=pk, h=h: e.tensor_tensor(out=S[:, h, :], in0=pk[:, :], in1=S[:, h, :], op=ALU.add), [pktok, "S"], ["S"])
            A("pool", lambda e, h=h: e.tensor_scalar(out=S[:, h, :], in0=S[:, h, :], scalar1=Gb[:, h:h + 1], scalar2=None, op0=ALU.mult), ["S", "Gb"], ["S"])
            A("pool", lambda e, h=h: e.tensor_copy(out=Sb[:, h, :], in_=S[:, h, :]), ["S"], ["Sb"])

        if upto <= 4:
            break
        A("dve", lambda e, c=c: e.tensor_scalar(out=dtv[:, :], in0=dt[:, :], scalar1=valid[:, c:c + 1], scalar2=None, op0=ALU.mult), ["dt", "valid"], ["dtv"])
        A("dve", lambda e: e.tensor_tensor(out=aa[:, :], in0=dt[:, :], in1=negA[:, :], op=ALU.mult), ["dt", "negA"], ["aa"])
        pm, pmtok = nextp()
        def f(e, pm=pm):
            e.matmul(pm[:, 0:16], lhsT=U[:, :], rhs=aa[:, :], start=True, stop=True)
            return e.matmul(pm[:, 16:32], lhsT=ones[:, :], rhs=aa[:, :], start=True, stop=True)
        A("pe", f, ["U", "ones", "aa"], [pmtok])
        A("act", lambda e, pm=pm: e.copy(out=acs[:, :], in_=pm[:, 0:16]), [pmtok], ["acs"])
        A("dve", lambda e, pm=pm: e.tensor_scalar(out=nacs[:, :], in0=pm[:, 0:16], scalar1=-1.0, scalar2=None, op0=ALU.mult), [pmtok], ["nacs"])
        A("act", lambda e, pm=pm: e.activation(out=cdec[:, :], in_=pm[:, 16:32], func=AF.Exp), [pmtok], ["cdec"])
        A("dve", lambda e, pm=pm: e.tensor_tensor(out=tmp16[:, :], in0=pm[:, 16:32], in1=acs[:, :], op=ALU.subtract), [pmtok, "acs"], ["tmp16"])
        A("act", lambda e: e.activation(out=te[:, :], in_=tmp16[:, :], func=AF.Exp), ["tmp16"], ["te"])
        A("act", lambda e: e.activation(out=el[:, :], in_=acs[:, :], func=AF.Exp), ["acs"], ["el"])
        def f(e):
            for k in range(8):
                i = e.transpose(out=pt[1][:, k * 128:(k + 1) * 128], in_=xc[:, k, :], identity=identb[:, :])
            return i
        A("pe", f, ["xc", "identb"], ["pt1"])
        A("act", lambda e: e.copy(out=xstok[:, :], in_=pt[1][:, :]), ["pt1"], ["xstok"])
        def f(e):
            for g in range(2):
                i = e.transpose(out=pt[0][:, g * 128:(g + 1) * 128], in_=xc[:, 8 + g, :], identity=identb[:, :])
            return i
        A("pe", f, ["xc", "identb"], ["pt0"])
        A("dve", lambda e: e.tensor_copy(out=Btok[:, :, :], in_=pt[0][:, 0:256].rearrange("p (g n) -> p g n", g=2)), ["pt0"], ["Btok"])
        A("dve", lambda e: e.tensor_tensor(out=v3(xdt[:, :], 16), in0=v3(xstok[:, :], 16), in1=bc(dtv[:, :].unsqueeze(2), [128, 16, 64]), op=ALU.mult), ["xstok", "dtv"], ["xdt"])
        A("dve", lambda e: e.tensor_tensor(out=v3(xdtw[:, :], 16), in0=v3(xdt[:, :], 16), in1=bc(te[:, :].unsqueeze(2), [128, 16, 64]), op=ALU.mult), ["xdt", "te"], ["xdtw"])
        if own:
            for g in range(2):
                p, ptok = nextp()
                A("pe", lambda e, p=p, g=g: e.matmul(p[:, 0:128], lhsT=xc[:, 8 + g, :], rhs=xc[:, 10 + g, :], start=True, stop=True), ["xc"], [ptok])
                A("act", lambda e, p=p, g=g: e.copy(out=cbt[:, g, :], in_=p[:, 0:128]), [ptok], ["cbt"])
            pyd = [(pg[4], "pg4"), (pg[5], "pg5")]
            for h in range(16):
                g = h // 8
                A("dve", lambda e, h=h: e.tensor_copy(out=abc[:, :], in_=bc(aa[:, h:h + 1], [128, 128])), ["aa"], ["abc"])
                p, ptok = nextp()
                def f(e, p=p):
                    e.matmul(p[:, 0:128], lhsT=abc[:, :], rhs=U[:, :], start=True, stop=False)
                    return e.matmul(p[:, 0:128], lhsT=identf[:, :], rhs=negm[:, :], start=False, stop=True)
                A("pe", f, ["abc", "U", "identf", "negm"], [ptok])
                A("act", lambda e, p=p, h=h: e.activation(out=dec[:, :], in_=p[:, 0:128], func=AF.Exp, bias=nacs[:, h:h + 1]), [ptok, "nacs"], ["dec"])
                A("pool", lambda e, g=g: e.tensor_tensor(out=Mb[:, :], in0=dec[:, :], in1=cbt[:, g, :], op=ALU.mult), ["dec", "cbt"], ["Mb"])
                A("pe", lambda e, h=h, g=g: e.matmul(pyd[g][0][:, (h % 8) * 64:(h % 8 + 1) * 64], lhsT=Mb[:, :], rhs=xdt[:, h * 64:(h + 1) * 64], start=True, stop=True), ["Mb", "xdt"], [pyd[g][1]])
            for g in range(2):
                gsl = slice(g * 512, (g + 1) * 512)
                po, potok = nextp()
                A("pe", lambda e, po=po, g=g, gsl=gsl: e.matmul(po[:, :], lhsT=xc[:, 10 + g, :], rhs=Hb[:, gsl], start=True, stop=True), ["xc", "Hb"], [potok])
                A("dve", lambda e, po=po, g=g, gsl=gsl: e.tensor_tensor(out=v3(ty[:, gsl], 8), in0=v3(po[:, :], 8), in1=bc(el[:, g * 8:(g + 1) * 8].unsqueeze(2), [128, 8, 64]), op=ALU.mult), [potok, "el"], ["ty"])
                A("dve", lambda e, g=g, gsl=gsl: e.tensor_tensor(out=ty[:, gsl], in0=ty[:, gsl], in1=pyd[g][0][:, :], op=ALU.add), ["ty", pyd[g][1]], ["ty"])
                A("pool", lambda e, g=g, gsl=gsl: e.tensor_tensor(out=v3(tu[:, :], 8), in0=v3(xstok[:, gsl], 8), in1=bc(dsk[:, g * 8:(g + 1) * 8].unsqueeze(2), [128, 8, 64]), op=ALU.mult), ["xstok", "dsk"], ["tu"])
                A("pool", lambda e, gsl=gsl: e.tensor_tensor(out=ty[:, gsl], in0=ty[:, gsl], in1=tu[:, :], op=ALU.add), ["ty", "tu"], ["ty"])
                A("pool", lambda e, gsl=gsl: e.tensor_tensor(out=ty[:, gsl], in0=ty[:, gsl], in1=zs[:, gsl], op=ALU.mult), ["ty", "zs"], ["ty"])
                A("act", lambda e, gsl=gsl: e.activation(out=tu[:, :], in_=ty[:, gsl], func=AF.Square, accum_out=ss2[:, 0:1]), ["ty"], ["ss2", "tu"])
                A("act", lambda e: e.activation(out=r2[:, :], in_=ss2[:, :], func=AF.Sqrt, scale=1.0 / 512, bias=EPS), ["ss2"], ["r2"])
                A("dve", lambda e: e.reciprocal(out=r2[:, :], in_=r2[:, :]), ["r2"], ["r2"])
                A("dve", lambda e, g=g, gsl=gsl: e.scalar_tensor_tensor(out=ycat[:, 1024 + g * 512:1024 + (g + 1) * 512], in0=ty[:, gsl], scalar=r2[:, 0:1], in1=snw[:, gsl], op0=ALU.mult, op1=ALU.mult), ["ty", "r2", "snw"], ["ycat"])
        for g in range(2):
            gsl = slice(g * 512, (g + 1) * 512)
            pn, pntok = nextp()
            A("pe", lambda e, pn=pn, g=g, gsl=gsl: e.matmul(pn[:, :], lhsT=Btok[:, g, :], rhs=xdtw[:, gsl], start=True, stop=True), ["Btok", "xdtw"], [pntok])
            A("dve", lambda e, g=g, gsl=gsl: e.tensor_tensor(out=v3(H[:, gsl], 8), in0=v3(H[:, gsl], 8), in1=bc(cdec[:, g * 8:(g + 1) * 8].unsqueeze(2), [128, 8, 64]), op=ALU.mult), ["H", "cdec"], ["H"])
            A("dve", lambda e, pn=pn, gsl=gsl: e.tensor_tensor(out=H[:, gsl], in0=H[:, gsl], in1=pn[:, :], op=ALU.add), ["H", pntok], ["H"])
            A("pool", lambda e, gsl=gsl: e.tensor_copy(out=Hb[:, gsl], in_=H[:, gsl]), ["H"], ["Hb"])
        if upto <= 5:
            break
        if not own:
            continue

        DUMP("ycat", ycat[:, :], "ycat", 2048)
        oc = c - OWN0
        A("dve", lambda e: e.tensor_copy(out=ycb[:, :], in_=ycat[:, :]), ["ycat"], ["xn"])
        transposes16(ycb, "xn", lambda half, oc=oc: yTall[:, half * 8:(half + 1) * 8, oc * 128:(oc + 1) * 128], "yTall")

    NOWN = TOK // 128
    if upto > 5 and chunks is None or (chunks is not None and upto > 5 and all(cc >= OWN0 for cc in chunks)):
        own_list = list(range(NOWN)) if chunks is None else [cc - OWN0 for cc in chunks]
        A("dve", lambda e: e.memset(bars["dve"][:, 0:1], 0.0), [], list(s.tk.keys()) + ["bar_all"])
        allbars = ["bar_all"]
        stackM.close()
        stackE = contextlib.ExitStack()
        SBE = lambda n, sh, dt=F32: stackE.enter_context(nc.sbuf_tensor("sbe_" + n, sh, dt))
        gE = SBE("gE", [128, D]); hE = SBE("hE", [128, NOWN, D]); hnE = SBE("hnE", [128, D])
        xnE = SBE("xnE", [128, D], BF16); lobE = SBE("lobE", [128, D], BF16); loTE = SBE("loTE", [128, 16, 128], BF16)
        comb = SBE("comb", [128, NOWN, 32]); sgl = SBE("sgl", [128, 512]); actb = SBE("actb", [128, 512], BF16)
        actT = SBE("actT", [128, NOWN, 4, 128], BF16)
        hnTall = yTall
        def BAR(eng, fn, reads, writes, **kw):
            return A(eng, fn, list(reads) + allbars, writes, **kw)
        BAR("sp", lambda e: e.dma_start(out=gE[:, :], in_=g2_d[0:1, :].partition_broadcast(128)), [], ["gE"], dma="gE")
        for j in own_list:
            BAR("sp", lambda e, j=j: e.dma_start(out=hE[:, j, :], in_=x_d[(OWN0 + j) * 128:(OWN0 + j + 1) * 128, :]), [], [f"hE{j}"], dma="xE")
        s.group_close("xE")
        for nb in range(4):
            nsl = slice(nb * 512, (nb + 1) * 512)
            wb, wtok = wload(wout_d[:, nsl].rearrange("(k p) n -> p k n", p=128), lambda b: b[:, :].rearrange("p (k n) -> p k n", k=16))
            w3 = wb[:, :].rearrange("p (k n) -> p k n", k=16)
            for j in own_list:
                p, ptok = nextp()
                def f(e, p=p, w3=w3, j=j):
                    for k in range(16):
                        i = e.matmul(p[:, :], lhsT=yTall[:, k, j * 128:(j + 1) * 128], rhs=w3[:, k, :], start=(k == 0), stop=(k == 15))
                    return i
                BAR("pe", f, [wtok, "yTall"], [ptok])
                BAR("dve", lambda e, p=p, nsl=nsl, j=j: e.tensor_tensor(out=hE[:, j, nsl], in0=hE[:, j, nsl], in1=p[:, :], op=ALU.add), [f"hE{j}", ptok], [f"hE{j}"])
        X = mybir.AxisListType.X
        for j in own_list:
            if dump == "h1" and j == own_list[0]:
                A("pool", lambda e, j=j: e.dma_start(out=dbg_d[:, :], in_=hE[:, j, :]), [f"hE{j}"], ["dbgd"], dma="dbg")
            jt = slice(j * 128, (j + 1) * 128)
            rms(hE[:, j, :], f"hE{j}", gE, "gE", hnE[:, :], "hnE", lobE[:, :], "lobE")
            BAR("dve", lambda e: e.tensor_copy(out=xnE[:, :], in_=hnE[:, :]), ["hnE"], ["xnE"])
            BAR("dve", lambda e: e.tensor_tensor(out=lobE[:, :], in0=hnE[:, :], in1=xnE[:, :], op=ALU.subtract), ["hnE", "xnE"], ["lobE"])
            transposes16(xnE, "xnE", lambda half, jt=jt: hnTall[:, half * 8:(half + 1) * 8, jt], "yTall")
            transposes16(lobE, "lobE", lambda half: loTE[:, half * 8:(half + 1) * 8, :], "loTE")
            p, ptok = nextp()
            def f(e, p=p, jt=jt):
                n = 0
                for (aT, w, sl) in ((hnTall, wrhi, jt), (loTE, wrhi, slice(0, 128)), (hnTall, wrlo, jt)):
                    for k in range(16):
                        i = e.matmul(p[:, 0:36], lhsT=aT[:, k, sl], rhs=w[:, k, :], start=(n == 0), stop=(n == 47))
                        n += 1
                return i
            A("pe", f, ["yTall", "loTE", "wrhi", "wrlo"], [ptok])
            A("dve", lambda e, p=p: e.tensor_tensor(out=lg[:, :], in0=p[:, 0:36], in1=rb[:, :], op=ALU.add), [ptok, "rb"], ["lg"])
            A("dve", lambda e: e.reduce_max(out=mx[:, :], in_=lg[:, 0:4], axis=X), ["lg"], ["mx"])
            A("dve", lambda e: e.tensor_scalar(out=ohg[:, :], in0=lg[:, 0:4], scalar1=mx[:, 0:1], scalar2=None, op0=ALU.is_equal), ["lg", "mx"], ["ohg"])
            A("dve", lambda e: e.tensor_scalar(out=nmx[:, :], in0=mx[:, :], scalar1=-1.0, scalar2=None, op0=ALU.mult), ["mx"], ["nmx"])
            A("act", lambda e: e.activation(out=eg[:, :], in_=lg[:, 0:4], func=AF.Exp, bias=nmx[:, 0:1], accum_out=sg[:, 0:1]), ["lg", "nmx"], ["eg", "sg"])
            A("dve", lambda e: e.reciprocal(out=psel[:, :], in_=sg[:, :]), ["sg"], ["psel"])
            A("dve", lambda e: e.tensor_scalar(out=les[:, :], in0=lg[:, 4:12], scalar1=ohg[:, 0:1], scalar2=None, op0=ALU.mult), ["lg", "ohg"], ["les"])
            for g in (1, 2, 3):
                A("dve", lambda e, g=g: e.scalar_tensor_tensor(out=les[:, :], in0=lg[:, 4 + 8 * g:12 + 8 * g], scalar=ohg[:, g:g + 1], in1=les[:, :], op0=ALU.mult, op1=ALU.add), ["lg", "ohg", "les"], ["les"])
            A("dve", lambda e: e.reduce_max(out=m1[:, :], in_=les[:, :], axis=X), ["les"], ["m1"])
            A("dve", lambda e: e.tensor_scalar(out=k1[:, :], in0=les[:, :], scalar1=m1[:, 0:1], scalar2=None, op0=ALU.is_equal), ["les", "m1"], ["k1"])
            A("dve", lambda e: e.scalar_tensor_tensor(out=les2[:, :], in0=k1[:, :], scalar=-1e30, in1=les[:, :], op0=ALU.mult, op1=ALU.add), ["k1", "les"], ["les2"])
            A("dve", lambda e: e.reduce_max(out=m2[:, :], in_=les2[:, :], axis=X), ["les2"], ["m2"])
            A("dve", lambda e: e.tensor_scalar(out=k2[:, :], in0=les2[:, :], scalar1=m2[:, 0:1], scalar2=None, op0=ALU.is_equal), ["les2", "m2"], ["k2"])
            A("dve", lambda e: e.tensor_tensor(out=dd[:, :], in0=m2[:, :], in1=m1[:, :], op=ALU.subtract), ["m1", "m2"], ["dd"])
            A("act", lambda e: e.activation(out=ed[:, :], in_=dd[:, :], func=AF.Exp), ["dd"], ["ed"])
            A("dve", lambda e: e.tensor_scalar(out=w1[:, :], in0=ed[:, :], scalar1=1.0, scalar2=None, op0=ALU.add), ["ed"], ["w1"])
            A("dve", lambda e: e.reciprocal(out=w1[:, :], in_=w1[:, :]), ["w1"], ["w1"])
            A("dve", lambda e: e.tensor_tensor(out=w2[:, :], in0=ed[:, :], in1=w1[:, :], op=ALU.mult), ["ed", "w1"], ["w2"])
            A("dve", lambda e: e.tensor_scalar(out=wig[:, :], in0=k1[:, :], scalar1=w1[:, 0:1], scalar2=None, op0=ALU.mult), ["k1", "w1"], ["wig"])
            A("dve", lambda e: e.scalar_tensor_tensor(out=wig[:, :], in0=k2[:, :], scalar=w2[:, 0:1], in1=wig[:, :], op0=ALU.mult, op1=ALU.add), ["k2", "w2", "wig"], ["wig"])
            for g in range(4):
                A("dve", lambda e, g=g: e.tensor_tensor(out=scg[:, :], in0=ohg[:, g:g + 1], in1=psel[:, :], op=ALU.mult), ["ohg", "psel"], ["scg"])
                BAR("dve", lambda e, g=g, j=j: e.tensor_scalar(out=comb[:, j, g * 8:(g + 1) * 8], in0=wig[:, :], scalar1=scg[:, 0:1], scalar2=None, op0=ALU.mult), ["wig", "scg"], ["comb"])
        if upto > 7:
            kview = lambda b: b[:, :].rearrange("p (k n) -> p k n", k=16)
            for ex in range(32):
                wgb, wgtok = wload(wg_d[ex].rearrange("(k p) n -> p k n", p=128), kview)
                wub, wutok = wload(wu_d[ex].rearrange("(k p) n -> p k n", p=128), kview)
                for j in own_list:
                    jt = slice(j * 128, (j + 1) * 128)
                    pgt, pgtok = nextp()
                    put, putok = nextp()
                    def f(e, pp=pgt, w3=kview(wgb), jt=jt):
                        for k in range(16):
                            i = e.matmul(pp[:, :], lhsT=hnTall[:, k, jt], rhs=w3[:, k, :], start=(k == 0), stop=(k == 15))
                        return i
                    A("pe", f, [wgtok, "yTall"], [pgtok])
                    def f(e, pp=put, w3=kview(wub), jt=jt):
                        for k in range(16):
                            i = e.matmul(pp[:, :], lhsT=hnTall[:, k, jt], rhs=w3[:, k, :], start=(k == 0), stop=(k == 15))
                        return i
                    A("pe", f, [wutok, "yTall"], [putok])
                    BAR("act", lambda e, pp=pgt: e.activation(out=sgl[:, :], in_=pp[:, :], func=AF.Silu), [pgtok], ["sgl"])
                    BAR("dve", lambda e, pp=put, ex=ex, j=j: e.scalar_tensor_tensor(out=actb[:, :], in0=sgl[:, :], scalar=comb[:, j, ex:ex + 1], in1=pp[:, :], op0=ALU.mult, op1=ALU.mult), ["sgl", "comb", putok], ["actb"])
                    def f(e):
                        for k in range(4):
                            i = e.transpose(out=pt[0][:, k * 128:(k + 1) * 128], in_=actb[:, k * 128:(k + 1) * 128], identity=identb[:, :])
                        return i
                    A("pe", f, ["actb", "identb"], ["pt0"])
                    BAR("act", lambda e, j=j: e.copy(out=actT[:, j, :, :], in_=pt[0][:, 0:512].rearrange("p (k t) -> p k t", k=4)), ["pt0"], [f"actT{j}"])
                wdb, wdtok = wload(wd_d[ex].rearrange("(k p) n -> p k n", p=128), lambda b: b[:, :].rearrange("p (k n) -> p k n", k=4))
                wd3 = wdb[:, :].rearrange("p (k n) -> p k n", k=4)
                for j in own_list:
                    for nb in range(4):
                        nsl = slice(nb * 512, (nb + 1) * 512)
                        pd, pdtok = nextp()
                        def f(e, pd=pd, wd3=wd3, nsl=nsl, j=j):
                            for k in range(4):
                                i = e.matmul(pd[:, :], lhsT=actT[:, j, k, :], rhs=wd3[:, k, nsl], start=(k == 0), stop=(k == 3))
                            return i
                        A("pe", f, [wdtok, f"actT{j}"], [pdtok])
                        A("dve", lambda e, pd=pd, nsl=nsl, j=j: e.tensor_tensor(out=hE[:, j, nsl], in0=hE[:, j, nsl], in1=pd[:, :], op=ALU.add), [f"hE{j}", pdtok], [f"hE{j}"])
            A("sp", lambda e: e.dma_start(out=gE[:, :], in_=gf_d[0:1, :].partition_broadcast(128)), [], ["gE"], dma="gE")
            for j in own_list:
                rms(hE[:, j, :], f"hE{j}", gE, "gE", hnE[:, :], "hnE", lobE[:, :], "lobE")
                A("sp", lambda e, j=j: e.dma_start(out=out_d[j * 128:(j + 1) * 128, :], in_=hnE[:, :]), ["hnE"], ["outd"], dma="out")
        s.emit()
        stackE.close()
        stackP.close()
        return nc

    s.emit()
    stackM.close()
    stackP.close()
    return nc


_NC_CACHE = {}


def kernel(x, positions, norm1_w, w_in, conv_w, conv_b, dt_bias, a_log, d_skip, ret_norm_w,
           ssm_norm_w, w_out, norm2_w, w_router_group, b_router_group, w_router_expert,
           b_router_expert, w_expert_gate, w_expert_up, w_expert_down, final_norm_w):
    f32 = np.float32
    x2 = np.asarray(x, f32).reshape(SEQ, D)
    pos = np.asarray(positions, np.int32).reshape(1, SEQ)
    idx = np.arange(128, dtype=np.float64)
    gam = 1.0 - 2.0 ** (-5.0 - np.arange(4, dtype=np.float64))
    rqv = (gam[:, None] ** (idx[None, :] + 1)).astype(f32).reshape(1, 512)
    rkv = ((gam[:, None] ** (-(idx[None, :] + 1))) * (256 ** -0.5)).astype(f32).reshape(1, 512)
    Gv = (gam ** 128).astype(f32).reshape(1, 4)
    invf = (10000.0 ** (-np.arange(128, dtype=f32) / 128)).astype(f32).reshape(128, 1)
    ident = np.eye(128, dtype=f32)
    si, li = np.meshgrid(np.arange(128), np.arange(128), indexing="ij")
    caus = (li >= si).astype(f32)
    Um = (si <= li).astype(f32)
    negm = np.where(li >= si, 0.0, -30000.0).astype(f32)
    shared = {
        "norm1_w": np.asarray(norm1_w, f32).reshape(1, D),
        "w_in": np.asarray(w_in, f32).reshape(D, INP),
        "conv_w": np.ascontiguousarray(np.asarray(conv_w, f32).reshape(4, 12, 128).transpose(2, 1, 0)),
        "conv_b": np.ascontiguousarray(np.asarray(conv_b, f32).reshape(12, 128).T),
        "dt_bias": np.asarray(dt_bias, f32).reshape(1, 16),
        "a_log": np.asarray(a_log, f32).reshape(1, 16),
        "d_skip": np.asarray(d_skip, f32).reshape(1, 16),
        "ret_norm_w": np.asarray(ret_norm_w, f32).reshape(1, 1024),
        "ssm_norm_w": np.asarray(ssm_norm_w, f32).reshape(1, 1024),
        "w_out": np.asarray(w_out, f32).reshape(D, D),
        "norm2_w": np.asarray(norm2_w, f32).reshape(1, D),
        "w_router": np.ascontiguousarray(np.concatenate([np.asarray(w_router_group, f32).reshape(D, 4), np.asarray(w_router_expert, f32).reshape(D, 32)], axis=1)),
        "b_router": np.concatenate([np.asarray(b_router_group, f32).reshape(1, 4), np.asarray(b_router_expert, f32).reshape(1, 32)], axis=1),
        "w_gate": np.asarray(w_expert_gate, f32).reshape(32, D, 512),
        "w_up": np.asarray(w_expert_up, f32).reshape(32, D, 512),
        "w_down": np.asarray(w_expert_down, f32).reshape(32, 512, D),
        "final_norm_w": np.asarray(final_norm_w, f32).reshape(1, D),
        "ident": ident, "caus": caus, "U": Um, "negm": negm, "rq": rqv, "rk": rkv, "G": Gv, "inv": invf,
    }
    in_maps = []
    for i in range(NCORE):
        n_real = TOK * (i + 1)
        xe = np.zeros((SEQ, D), f32)
        xe[SEQ - n_real:] = x2[:n_real]
        pe_ = np.zeros((1, SEQ), np.int32)
        pe_[0, SEQ - n_real:] = pos[0, :n_real]
        val = np.zeros(SEQ, f32)
        val[SEQ - n_real:] = 1.0
        m = dict(shared)
        m["x"] = xe
        m["pos"] = pe_
        m["valid"] = np.ascontiguousarray(val.reshape(NCH, 128).T)
        in_maps.append(m)
    if "nc" not in _NC_CACHE:
        _NC_CACHE["nc"] = build_nc()
    res = run_bass_kernel_spmd(_NC_CACHE["nc"], in_maps, core_ids=list(range(NCORE)))
    out = np.concatenate([np.asarray(r["out"], f32) for r in res.results], axis=0)
    return out.reshape(1, SEQ, D)
```

```python
import contextlib
import numpy as np
import concourse.bass as bass
import concourse.mybir as mybir
from concourse.bass_utils import run_bass_kernel_spmd

F32 = mybir.dt.float32
BF16 = mybir.dt.bfloat16
I32 = mybir.dt.int32
AF = mybir.ActivationFunctionType
ALU = mybir.AluOpType
ENGS = ("pe", "act", "dve", "pool", "sp")
NCORE = 8
D = 2048
SEQ = 8192
TOK = SEQ // NCORE
NCH = SEQ // 128
OWN0 = NCH - TOK // 128
INP = 6672
EPS = 1e-6


class Tok:
    __slots__ = ("name", "writer", "readers")

    def __init__(self, name):
        self.name = name
        self.writer = None
        self.readers = []


class Op:
    __slots__ = ("eng", "fn", "deps", "dma_sem", "dma_val", "signal", "sig_val", "cc")


class Sched:
    def __init__(self, nc):
        self.nc = nc
        self.ops = {e: [] for e in ENGS}
        self.dma_counts = {}
        self.tk = {}
        self.closed = {}

    def T(self, name):
        t = self.tk.get(name)
        if t is None:
            t = self.tk[name] = Tok(name)
        return t

    def add(self, eng, fn, reads=(), writes=(), dma=None, cc=False):
        op = Op()
        op.cc = cc
        op.eng, op.fn, op.deps, op.signal, op.sig_val = eng, fn, [], False, None
        op.dma_sem, op.dma_val = dma, None
        if dma is not None:
            c = self.dma_counts.get(dma, 0) + (1 if cc else 16)
            self.dma_counts[dma] = c
            op.dma_val = c
        deps = {}
        rt = [self.T(x) for x in reads]
        wt = [self.T(x) for x in writes]
        for t in rt:
            if t.writer is not None:
                deps[id(t.writer)] = t.writer
        for t in wt:
            if t.writer is not None:
                deps[id(t.writer)] = t.writer
            for r in t.readers:
                deps[id(r)] = r
        for d in deps.values():
            if d.dma_sem is None and d.eng == "pe" and eng == "pe" and dma is None:
                continue
            op.deps.append(d)
            if d.dma_sem is None:
                d.signal = True
        for t in rt:
            t.readers.append(op)
        for t in wt:
            t.writer = op
            t.readers = []
        self.ops[eng].append(op)
        return op

    def group_close(self, dma):
        tot = self.dma_counts[dma]
        done = self.closed.setdefault(dma, set())
        for e in ENGS:
            for op in self.ops[e]:
                if op.dma_sem == dma and id(op) not in done:
                    op.dma_val = tot
                    done.add(id(op))

    def emit(self):
        nc = self.nc
        with contextlib.ExitStack() as stack:
            block = stack.enter_context(nc.Block())
            esem = {e: stack.enter_context(nc.semaphore(f"s_{e}")) for e in ENGS if e != "sp"}
            dsem = {k: stack.enter_context(nc.semaphore(f"d_{k}")) for k in self.dma_counts}
            for e in ENGS:
                c = 0
                for op in self.ops[e]:
                    if op.dma_sem is None and op.signal:
                        c += 1
                        op.sig_val = c
            ops = self.ops
            counts = self.dma_counts

            def run(e, engobj):
                waited = {}
                for op in ops[e]:
                    for d in op.deps:
                        if d.dma_sem is not None:
                            key, sem, val = ("d", d.dma_sem), dsem[d.dma_sem], d.dma_val
                        else:
                            key, sem, val = ("e", d.eng), esem[d.eng], d.sig_val
                        if waited.get(key, 0) >= val:
                            continue
                        waited[key] = val
                        engobj.wait_ge(sem, val)
                    ins = op.fn(engobj)
                    if op.dma_sem is not None:
                        if op.cc:
                            ins.then_inc(dsem[op.dma_sem])
                        else:
                            ins.then_inc(dsem[op.dma_sem], 16)
                    elif op.signal:
                        ins.then_inc(esem[e], 1)
                if e == "sp":
                    for k, c in counts.items():
                        engobj.wait_ge(dsem[k], c)

            @block.tensor
            def _(pe):
                run("pe", pe)

            @block.scalar
            def _(act):
                run("act", act)

            @block.vector
            def _(dve):
                run("dve", dve)

            @block.gpsimd
            def _(pool):
                run("pool", pool)

            @block.sync
            def _(sp):
                run("sp", sp)


def build_nc(chunks=None, upto=99, dump=None, nocc=False):
    nc = bass.Bass("TRN2", target_bir_lowering=False)
    DI = lambda n, sh, dt=F32: nc.dram_tensor(n, sh, dt, kind="ExternalInput").ap()
    XR = TOK + 128
    x_d = DI("x", [XR, D])
    pos_d = DI("pos", [1, XR], I32)
    cR_d = DI("cR", [1, 32])
    Mi_d = DI("Mi", [1, 64])
    vi_d = DI("vi", [1, 8])
    g1_d = DI("norm1_w", [1, D])
    win_d = DI("w_in", [D, INP])
    cw_d = DI("conv_w", [128, 12, 4])
    cb_d = DI("conv_b", [128, 12])
    dtb_d = DI("dt_bias", [1, 16])
    alog_d = DI("a_log", [1, 16])
    dsk_d = DI("d_skip", [1, 16])
    rnw_d = DI("ret_norm_w", [1, 1024])
    snw_d = DI("ssm_norm_w", [1, 1024])
    wout_d = DI("w_out", [D, D])
    g2_d = DI("norm2_w", [1, D])
    wr_d = DI("w_router", [D, 36])
    rb_d = DI("b_router", [1, 36])
    wg_d = DI("w_gate", [32, D, 512])
    wu_d = DI("w_up", [32, D, 512])
    wd_d = DI("w_down", [32, 512, D])
    gf_d = DI("final_norm_w", [1, D])
    ident_d = DI("ident", [128, 128])
    caus_d = DI("caus", [128, 128])
    U_d = DI("U", [128, 128])
    negm_d = DI("negm", [128, 128])
    rq_d = DI("rq", [1, 512])
    rk_d = DI("rk", [1, 512])
    G_d = DI("G", [1, 4])
    inv_d = DI("inv", [128, 1])
    out_d = nc.dram_tensor("out", [TOK, D], F32, kind="ExternalOutput").ap()
    dbg_d = nc.dram_tensor("dbg", [128, D], F32, kind="ExternalOutput").ap() if dump else None

    s = Sched(nc)
    stackP = contextlib.ExitStack()
    stackM = contextlib.ExitStack()
    SBP = lambda n, sh, dt=F32: stackP.enter_context(nc.sbuf_tensor("sb_" + n, sh, dt))
    PI = float(np.pi)
    NW = 2
    wts = [SBP(f"wt{i}", [128, 16 * 512], BF16) for i in range(NW)]
    yTall = SBP("yTall", [128, 16, TOK], BF16)
    identb = SBP("identb", [128, 128], BF16)
    rb = SBP("rb", [128, 36]); wrhi = SBP("wrhi", [128, 16, 36], BF16); wrlo = SBP("wrlo", [128, 16, 36], BF16)
    ss = SBP("ss", [128, 1]); rstd = SBP("rstd", [128, 1])
    lg = SBP("lg", [128, 36]); mx = SBP("mx", [128, 1]); nmx = SBP("nmx", [128, 1]); ohg = SBP("ohg", [128, 4])
    eg = SBP("eg", [128, 4]); sg = SBP("sg", [128, 1]); psel = SBP("psel", [128, 1])
    les = SBP("les", [128, 8]); les2 = SBP("les2", [128, 8]); k1 = SBP("k1", [128, 8]); k2 = SBP("k2", [128, 8])
    m1 = SBP("m1", [128, 1]); m2 = SBP("m2", [128, 1]); dd = SBP("dd", [128, 1]); ed = SBP("ed", [128, 1]); w1 = SBP("w1", [128, 1]); w2 = SBP("w2", [128, 1])
    wig = SBP("wig", [128, 8]); scg = SBP("scg", [128, 1])
    bars = {e: SBP(f"bar_{e}", [128, 2]) for e in ("act", "dve", "pool")}
    pg = [stackP.enter_context(nc.psum_tensor(f"pg{i}", [128, 512], F32)) for i in range(6)]
    pt = [stackP.enter_context(nc.psum_tensor(f"pt{i}", [128, 1024], BF16)) for i in range(2)]
    SB = lambda n, sh, dt=F32: stackM.enter_context(nc.sbuf_tensor("sb_" + n, sh, dt))

    xin = SB("xin", [128, D]); xn = SB("xn", [128, D], BF16)
    g1 = SB("g1", [128, D])
    rnw = SB("rnw", [128, 1024]); snw = SB("snw", [128, 1024])
    xnT = SB("xnT", [128, 16, 128], BF16)
    identf = SB("identf", [128, 128])
    causb = SB("causb", [128, 128], BF16); causf = SB("causf", [128, 128])
    U = SB("U", [128, 128]); negm = SB("negm", [128, 128]); ones = SB("ones", [128, 128])
    rq = SB("rq", [128, 4, 128]); rk = SB("rk", [128, 4, 128]); Gb = SB("Gb", [128, 4])
    inv = SB("inv", [128, 1]); cR = SB("cR", [128, 32]); Mi = SB("Mi", [128, 8, 8]); vi = SB("vi", [128, 8])
    halo = SB("halo", [128, 12, 3]); Atot = SB("Atot", [128, 16]); Aall = SB("Aall", [128, 8, 16]); cH = SB("cH", [128, 8, 16]); tmpA = SB("tmpA", [128, 8, 16])
    gbuf = SB("gbuf", [128, 3088])
    cw = SB("cw", [128, 12, 4]); cb = SB("cb", [128, 12])
    dtb = SB("dtb", [128, 16]); negA = SB("negA", [128, 16]); dsk = SB("dsk", [128, 16])
    wr = SB("wr", [128, 16, 36])
    posi = SB("posi", [128, 128], I32); pf = SB("pf", [128, 128]); ang = SB("ang", [128, 128])
    ra = SB("ra", [128, 128]); ki = SB("ki", [128, 128], I32); kf = SB("kf", [128, 128]); mk = SB("mk", [128, 128])
    cos = SB("cos", [128, 128]); sin = SB("sin", [128, 128])
    rt = [SB(f"rt{i}", [128, 128]) for i in range(6)]
    qT = SB("qT", [128, 8, 128], BF16); kT = SB("kT", [128, 8, 128], BF16)
    v = SB("v", [128, 1024], BF16); gs = SB("gs", [128, 1024]); zs = SB("zs", [128, 1024])
    xbc = SB("xbc", [128, 12, 131]); xc = SB("xc", [128, 12, 128], BF16)
    cvo = [SB(f"cvo{i}", [128, 128]) for i in range(2)]
    dtr = SB("dtr", [128, 16]); dt = SB("dt", [128, 16]); dtv = SB("dtv", [128, 16]); aa = SB("aa", [128, 16])
    acs = SB("acs", [128, 16]); nacs = SB("nacs", [128, 16]); cdec = SB("cdec", [128, 16]); te = SB("te", [128, 16]); el = SB("el", [128, 16])
    tmp16 = SB("tmp16", [128, 16])
    ktok = SB("ktok", [128, 1024], BF16); xstok = SB("xstok", [128, 1024], BF16); Btok = SB("Btok", [128, 2, 128], BF16)
    scm = SB("scm", [128, 128], BF16)
    st6 = SB("st6", [128, 6]); mv = SB("mv", [128, 2]); rs = SB("rs", [128, 1])
    yn = SB("yn", [128, 256]); ycat = SB("ycat", [128, D]); ycb = xn; junk = xn
    S = SB("S", [128, 4, 512]); Sb = SB("Sb", [128, 4, 512], BF16)
    H = SB("H", [128, 1024]); Hb = SB("Hb", [128, 1024], BF16)
    xdt = SB("xdt", [128, 1024], BF16); xdtw = SB("xdtw", [128, 1024], BF16)
    cbt = SB("cbt", [128, 2, 128]); abc = SB("abc", [128, 128]); dec = SB("dec", [128, 128]); Mb = SB("Mb", [128, 128], BF16)
    ty = SB("ty", [128, 1024]); tu = SB("tu", [128, 512]); ss2 = SB("ss2", [128, 1]); r2 = SB("r2", [128, 1])
    pcnt = [0]

    def nextp():
        i = pcnt[0] % 4
        pcnt[0] += 1
        return pg[i], f"pg{i}"

    wb_in = nc.dram_tensor("wb_in", [14, D, 512], BF16)
    wb_out = nc.dram_tensor("wb_out", [4, D, 512], BF16)
    wb_g = nc.dram_tensor("wb_g", [32, D, 512], BF16)
    wb_u = nc.dram_tensor("wb_u", [32, D, 512], BF16)
    wb_d = nc.dram_tensor("wb_d", [32, 512, D], BF16)
    wcnt = [0]

    def wload(src_ap, view):
        i = wcnt[0] % NW
        wcnt[0] += 1
        buf = wts[i]
        s.add("sp", lambda e: e.dma_start(out=view(buf), in_=src_ap), reads=[src_tok[0]], writes=[f"wt{i}"], dma=f"w{i}")
        return buf, f"wt{i}"

    src_tok = ["wb_in13"]

    def A(eng, fn, reads=(), writes=(), dma=None, cc=False):
        if eng == "pool" and dma is None:
            eng = "dve"
        return s.add(eng, fn, reads, writes, dma=dma, cc=cc)

    def DUMP(name, ap, tok, width):
        if dump == name:
            A("pool", lambda e: e.dma_start(out=dbg_d[:, 0:width], in_=ap), [tok], ["dbgd"], dma="dbg")
    bc = lambda ap, sh: ap.to_broadcast(sh)

    def cload(buf_ap, src, tok):
        A("sp", lambda e: e.dma_start(out=buf_ap, in_=src), writes=[tok], dma="const")
    cload(g1[:, :], g1_d[0:1, :].partition_broadcast(128), "g1")
    cload(rnw[:, :], rnw_d[0:1, :].partition_broadcast(128), "rnw")
    cload(snw[:, :], snw_d[0:1, :].partition_broadcast(128), "snw")
    cload(identf[:, :], ident_d[:, :], "identf")
    cload(causf[:, :], caus_d[:, :], "causf")
    cload(U[:, :], U_d[:, :], "U")
    cload(negm[:, :], negm_d[:, :], "negm")
    cload(rq[:, :, :].rearrange("p h l -> p (h l)"), rq_d[0:1, :].partition_broadcast(128), "rq")
    cload(rk[:, :, :].rearrange("p h l -> p (h l)"), rk_d[0:1, :].partition_broadcast(128), "rk")
    cload(Gb[:, :], G_d[0:1, :].partition_broadcast(128), "Gb")
    cload(inv[:, :], inv_d[:, :], "inv")
    cload(cR[:, :], cR_d[0:1, :].partition_broadcast(128), "cR")
    cload(Mi[:, :, :].rearrange("p a b -> p (a b)"), Mi_d[0:1, :].partition_broadcast(128), "Mi")
    cload(vi[:, :], vi_d[0:1, :].partition_broadcast(128), "vi")
    cload(cw[:, :, :], cw_d[:, :, :], "cw")
    cload(cb[:, :], cb_d[:, :], "cb")
    cload(dtb[:, :], dtb_d[0:1, :].partition_broadcast(128), "dtb")
    cload(negA[:, :], alog_d[0:1, :].partition_broadcast(128), "negA")
    cload(dsk[:, :], dsk_d[0:1, :].partition_broadcast(128), "dsk")
    cload(wr[:, :, :], wr_d.rearrange("(k p) n -> p k n", p=128), "wr")
    cload(rb[:, :], rb_d[0:1, :].partition_broadcast(128), "rb")
    s.group_close("const")
    for t in range(14):
        wc = 512 if t < 13 else 16
        s.add("pool", lambda e, t=t, wc=wc: e.dma_start(out=wb_in.ap()[t, :, 0:wc], in_=win_d[:, t * 512:t * 512 + wc]), writes=[f"wb_in{t}"], dma="cv")
    s.group_close("cv")
    for nb in range(4):
        s.add("pool", lambda e, nb=nb: e.dma_start(out=wb_out.ap()[nb, :, :], in_=wout_d[:, nb * 512:(nb + 1) * 512]), reads=["wb_in13"], writes=[f"wb_out{nb}"], dma="cv")
    s.group_close("cv")
    for ex in range(32):
        s.add("pool", lambda e, ex=ex: e.dma_start(out=wb_g.ap()[ex, :, :], in_=wg_d[ex]), reads=["wb_out3"], writes=[f"wb_g{ex}"], dma="cv")
        s.add("pool", lambda e, ex=ex: e.dma_start(out=wb_u.ap()[ex, :, :], in_=wu_d[ex]), writes=[f"wb_u{ex}"], dma="cv")
        s.add("pool", lambda e, ex=ex: e.dma_start(out=wb_d.ap()[ex, :, :], in_=wd_d[ex]), writes=[f"wb_d{ex}"], dma="cv")
    s.group_close("cv")
    A("dve", lambda e: e.tensor_copy(out=identb[:, :], in_=identf[:, :]), ["identf"], ["identb"])
    A("dve", lambda e: e.tensor_copy(out=causb[:, :], in_=causf[:, :]), ["causf"], ["causb"])
    A("dve", lambda e: e.memset(ones[:, :], 1.0), [], ["ones"])
    A("dve", lambda e: e.tensor_copy(out=wrhi[:, :, :], in_=wr[:, :, :]), ["wr"], ["wrhi"])
    A("dve", lambda e: e.tensor_tensor(out=wrlo[:, :, :], in0=wr[:, :, :], in1=wrhi[:, :, :], op=ALU.subtract), ["wr", "wrhi"], ["wrlo"])
    A("act", lambda e: e.activation(out=negA[:, :], in_=negA[:, :], func=AF.Exp), ["negA"], ["negA"])
    A("dve", lambda e: e.tensor_scalar(out=negA[:, :], in0=negA[:, :], scalar1=-1.0, scalar2=None, op0=ALU.mult), ["negA"], ["negA"])
    A("dve", lambda e: e.memset(xbc[:, :, :], 0.0), [], ["xbc"])
    A("dve", lambda e: e.memset(S[:, :, :], 0.0), [], ["S"])
    A("dve", lambda e: e.memset(Sb[:, :, :], 0.0), [], ["Sb"])
    A("dve", lambda e: e.memset(H[:, :], 0.0), [], ["H"])
    A("dve", lambda e: e.memset(Hb[:, :], 0.0), [], ["Hb"])
    A("dve", lambda e: e.memset(Atot[:, :], 0.0), [], ["Atot"])

    def rms(src, srctok, wbuf, wtok, dst, dsttok, jk, jktok):
        A("act", lambda e: e.activation(out=jk, in_=src, func=AF.Square, accum_out=ss[:, 0:1]), [srctok], ["ss", jktok])
        A("act", lambda e: e.activation(out=rstd[:, :], in_=ss[:, :], func=AF.Sqrt, scale=1.0 / D, bias=EPS), ["ss"], ["rstd"])
        A("dve", lambda e: e.reciprocal(out=rstd[:, :], in_=rstd[:, :]), ["rstd"], ["rstd"])
        A("dve", lambda e: e.scalar_tensor_tensor(out=dst, in0=src, scalar=rstd[:, 0:1], in1=wbuf[:, :], op0=ALU.mult, op1=ALU.mult), [srctok, "rstd", wtok], [dsttok])

    def transposes16(src, srctok, dstf, dsttok):
        for half in range(2):
            p = pt[half]
            def f(e, half=half, p=p):
                for k in range(8):
                    kk = half * 8 + k
                    i = e.transpose(out=p[:, k * 128:(k + 1) * 128], in_=src[:, kk * 128:(kk + 1) * 128], identity=identb[:, :])
                return i
            A("pe", f, [srctok, "identb"], [f"pt{half}"])
            if half == 0:
                A("act", lambda e, half=half, p=p: e.copy(out=dstf(half), in_=p[:, :].rearrange("p (k t) -> p k t", k=8)), [f"pt{half}"], [dsttok])
            else:
                A("dve", lambda e, half=half, p=p: e.tensor_copy(out=dstf(half), in_=p[:, :].rearrange("p (k t) -> p k t", k=8)), [f"pt{half}"], [dsttok])

    def sincos(shift, outbuf, outtok):
        A("dve", lambda e: e.tensor_scalar(out=ra[:, :], in0=ang[:, :], scalar1=float(shift), scalar2=None, op0=ALU.add), ["ang"], ["ra"])
        A("dve", lambda e: e.tensor_scalar(out=ki[:, :], in0=ra[:, :], scalar1=float(1 / (2 * PI)), scalar2=None, op0=ALU.mult), ["ra"], ["ki"])
        A("dve", lambda e: e.tensor_copy(out=kf[:, :], in_=ki[:, :]), ["ki"], ["kf"])
        A("dve", lambda e: e.scalar_tensor_tensor(out=ra[:, :], in0=kf[:, :], scalar=float(-2 * PI), in1=ra[:, :], op0=ALU.mult, op1=ALU.add), ["kf", "ra"], ["ra"])
        A("dve", lambda e: e.tensor_scalar(out=mk[:, :], in0=ra[:, :], scalar1=PI, scalar2=float(-2 * PI), op0=ALU.is_gt, op1=ALU.mult), ["ra"], ["mk"])
        A("dve", lambda e: e.tensor_tensor(out=ra[:, :], in0=ra[:, :], in1=mk[:, :], op=ALU.add), ["ra", "mk"], ["ra"])
        A("dve", lambda e: e.tensor_scalar_max(out=ra[:, :], in0=ra[:, :], scalar1=-3.1415925), ["ra"], ["ra"])
        A("dve", lambda e: e.tensor_scalar_min(out=ra[:, :], in0=ra[:, :], scalar1=3.1415925), ["ra"], ["ra"])
        A("act", lambda e: e.activation(out=outbuf[:, :], in_=ra[:, :], func=AF.Sin), ["ra"], [outtok])

    v3 = lambda ap, h: ap.rearrange("p (h j) -> p h j", h=h)

    NOWN = TOK // 128
    own_ids = list(range(NOWN)) if chunks is None else list(chunks)
    visits = [("halo", -1)] + [("state", j) for j in own_ids] + [("cc", 0)] + [("own", j) for j in own_ids]
    cc_in = [nc.dram_tensor(f"cc_in{i}", [128, 1040], F32) for i in range(3)]
    cc_out = [nc.dram_tensor(f"cc_out{i}", [NCORE * 128, 1040], F32) for i in range(3)]
    for (mode, jv) in visits:
        if mode == "cc":
            if nocc:
                A("dve", lambda e: e.memset(S[:, :, :], 0.0), ["S"], ["S"])
                A("dve", lambda e: e.memset(Sb[:, :, :], 0.0), ["Sb"], ["Sb"])
                A("dve", lambda e: e.memset(H[:, :], 0.0), ["H"], ["H"])
                A("dve", lambda e: e.memset(Hb[:, :], 0.0), ["Hb"], ["Hb"])
            else:
                Sflat = S[:, :, :].rearrange("p a b -> p (a b)")
                A("sp", lambda e: e.dma_start(out=cc_in[0].ap()[:, 0:1024], in_=Sflat[:, 0:1024]), ["S"], ["ccin0"], dma="misc")
                A("sp", lambda e: e.dma_start(out=cc_in[1].ap()[:, 0:1024], in_=Sflat[:, 1024:2048]), ["S"], ["ccin1"], dma="misc")
                A("sp", lambda e: e.dma_start(out=cc_in[2].ap()[:, 0:1024], in_=H[:, :]), ["H"], ["ccin2"], dma="misc")
                A("sp", lambda e: e.dma_start(out=cc_in[2].ap()[:, 1024:1040], in_=Atot[:, :]), ["Atot"], ["ccin2b"], dma="misc")
                s.group_close("misc")
                for i in range(3):
                    A("pool", lambda e, i=i: e.collective_compute("AllGather", ALU.bypass, replica_groups=[list(range(NCORE))], ins=[cc_in[i].ap().opt()], outs=[cc_out[i].ap().opt()]),
                      ["ccin0", "ccin1", "ccin2", "ccin2b", "wb_in13", "wb_out3", "wb_d31"], ["ccout", "ccchain"], dma="cc", cc=True)
                ccv = [cc_out[i].ap().rearrange("(r p) f -> p r f", p=128) for i in range(3)]
                A("sp", lambda e: e.dma_start(out=Aall[:, :, :], in_=ccv[2][:, :, 1024:1040]), ["ccout"], ["Aall"], dma="misc")
                s.group_close("misc")
                A("dve", lambda e: e.memset(S[:, :, :], 0.0), ["S"], ["S"])
                A("dve", lambda e: e.memset(H[:, :], 0.0), ["H"], ["H"])
                for m in range(NCORE):
                    A("dve", lambda e, m=m: e.tensor_tensor(out=tmpA[:, :, :], in0=Aall[:, :, :], in1=bc(Mi[:, m, :].unsqueeze(2), [128, 8, 16]), op=ALU.mult), ["Aall", "Mi"], ["tmpA"])
                    A("dve", lambda e, m=m: e.reduce_sum(out=cH[:, m, :], in_=tmpA[:, :, :].rearrange("p r h -> p h r"), axis=mybir.AxisListType.X), ["tmpA"], ["cH"])
                    A("act", lambda e, m=m: e.activation(out=cH[:, m, :], in_=cH[:, m, :], func=AF.Exp), ["cH"], ["cH"])
                    A("dve", lambda e, m=m: e.tensor_scalar(out=cH[:, m, :], in0=cH[:, m, :], scalar1=vi[:, m:m + 1], scalar2=None, op0=ALU.mult), ["cH", "vi"], ["cH"])
                for m in range(NCORE):
                    A("sp", lambda e, m=m: e.dma_start(out=gbuf[:, 0:1024], in_=ccv[0][:, m, 0:1024]), ["ccout"], ["gbuf"], dma="misc")
                    A("sp", lambda e, m=m: e.dma_start(out=gbuf[:, 1024:2048], in_=ccv[1][:, m, 0:1024]), ["ccout"], ["gbuf2"], dma="misc")
                    A("sp", lambda e, m=m: e.dma_start(out=gbuf[:, 2048:3072], in_=ccv[2][:, m, 0:1024]), ["ccout"], ["gbuf3"], dma="misc")
                    s.group_close("misc")
                    for h in range(4):
                        A("dve", lambda e, m=m, h=h: e.scalar_tensor_tensor(out=S[:, h, :], in0=gbuf[:, h * 512:(h + 1) * 512], scalar=cR[:, m * 4 + h:m * 4 + h + 1], in1=S[:, h, :], op0=ALU.mult, op1=ALU.add), ["gbuf", "gbuf2", "cR", "S"], ["S"])
                    A("pool", lambda e, m=m: e.tensor_tensor(out=v3(ty[:, :], 16), in0=v3(gbuf[:, 2048:3072], 16), in1=bc(cH[:, m, :].unsqueeze(2), [128, 16, 64]), op=ALU.mult), ["gbuf3", "cH"], ["ty"])
                    A("pool", lambda e: e.tensor_tensor(out=H[:, :], in0=H[:, :], in1=ty[:, :], op=ALU.add), ["H", "ty"], ["H"])
                A("pool", lambda e: e.tensor_copy(out=Sb[:, :, :], in_=S[:, :, :]), ["S"], ["Sb"])
                A("pool", lambda e: e.tensor_copy(out=Hb[:, :], in_=H[:, :]), ["H"], ["Hb"])
            A("dve", lambda e: e.tensor_copy(out=xbc[:, :, 0:3], in_=halo[:, :, :]), ["halo", "xbc"], ["xbc"])
            continue
        own = mode == "own"
        c = jv + 1
        cs = slice(c * 128, (c + 1) * 128)
        A("sp", lambda e, cs=cs: e.dma_start(out=xin[:, :], in_=x_d[cs, :]), [], ["xin"], dma="x")
        A("sp", lambda e, cs=cs: e.dma_start(out=posi[:, :], in_=pos_d[0:1, cs].partition_broadcast(128)), [], ["posi"], dma="pos")
        rms(xin[:, :], "xin", g1, "g1", xn[:, :], "xn", junk[:, :], "xn")
        transposes16(xn, "xn", lambda half: xnT[:, half * 8:(half + 1) * 8, :], "xnT")
        A("dve", lambda e: e.tensor_copy(out=pf[:, :], in_=posi[:, :]), ["posi"], ["pf"])
        A("dve", lambda e: e.tensor_scalar(out=ang[:, :], in0=pf[:, :], scalar1=inv[:, 0:1], scalar2=None, op0=ALU.mult), ["pf", "inv"], ["ang"])
        sincos(0.0, sin, "sin")
        sincos(PI / 2, cos, "cos")

        if upto <= 1:
            break
        DUMP("xn", xn[:, :], "xn", 2048)
        DUMP("cos", cos[:, :], "cos", 128)
        DUMP("sin", sin[:, :], "sin", 128)
        tiles = list(range(14)) if own else ([10, 11, 12] if mode == "halo" else [2, 3, 4, 5, 10, 11, 12, 13])
        for t in tiles:
            wcols = 512 if t < 13 else 16
            src_tok[0] = "wb_in13"
            wb, wtok = wload(wb_in.ap()[t].rearrange("(k p) n -> p k n", p=128)[:, :, 0:wcols],
                             lambda b, wcols=wcols: b[:, :].rearrange("p (k n) -> p k n", k=16)[:, :, 0:wcols])
            w3 = wb[:, :].rearrange("p (k n) -> p k n", k=16)
            if t in (0, 1, 2, 3, 10, 11, 12):
                p, ptok = nextp()
                def f(e, p=p, w3=w3):
                    for b in range(4):
                        for k in range(16):
                            i = e.matmul(p[:, b * 128:(b + 1) * 128], lhsT=w3[:, k, b * 128:(b + 1) * 128], rhs=xnT[:, k, :], start=(k == 0), stop=(k == 15))
                    return i
                A("pe", f, [wtok, "xnT"], [ptok])
                if t >= 10:
                    A("act", lambda e, p=p, t=t: e.copy(out=xbc[:, (t - 10) * 4:(t - 10) * 4 + 4, 3:131], in_=p[:, :].rearrange("p (b t) -> p b t", b=4)), [ptok], ["xbc"])
                else:
                    isq = t < 2
                    dstT, dtok, tbl, tbtok = (qT, "qT", rq, "rq") if isq else (kT, "kT", rk, "rk")
                    for hh in range(2):
                        h = (t % 2) * 2 + hh
                        x1 = p[:, (2 * hh) * 128:(2 * hh + 1) * 128]
                        x2 = p[:, (2 * hh + 1) * 128:(2 * hh + 2) * 128]
                        A("dve", lambda e, x1=x1: e.tensor_tensor(out=rt[0][:, :], in0=x1, in1=cos[:, :], op=ALU.mult), [ptok, "cos"], ["rt0"])
                        A("dve", lambda e, x2=x2: e.tensor_tensor(out=rt[1][:, :], in0=x2, in1=sin[:, :], op=ALU.mult), [ptok, "sin"], ["rt1"])
                        A("dve", lambda e, x2=x2: e.tensor_tensor(out=rt[2][:, :], in0=x2, in1=cos[:, :], op=ALU.mult), [ptok, "cos"], ["rt2"])
                        A("dve", lambda e, x1=x1: e.tensor_tensor(out=rt[3][:, :], in0=x1, in1=sin[:, :], op=ALU.mult), [ptok, "sin"], ["rt3"])
                        A("pool", lambda e: e.tensor_tensor(out=rt[4][:, :], in0=rt[0][:, :], in1=rt[1][:, :], op=ALU.subtract), ["rt0", "rt1"], ["rt4"])
                        A("pool", lambda e: e.tensor_tensor(out=rt[5][:, :], in0=rt[2][:, :], in1=rt[3][:, :], op=ALU.add), ["rt2", "rt3"], ["rt5"])
                        A("pool", lambda e, h=h, dstT=dstT, tbl=tbl: e.tensor_tensor(out=dstT[:, 2 * h, :], in0=rt[4][:, :], in1=tbl[:, h, :], op=ALU.mult), ["rt4", tbtok], [dtok])
                        A("pool", lambda e, h=h, dstT=dstT, tbl=tbl: e.tensor_tensor(out=dstT[:, 2 * h + 1, :], in0=rt[5][:, :], in1=tbl[:, h, :], op=ALU.mult), ["rt5", tbtok], [dtok])
            else:
                p, ptok = nextp()
                def f(e, p=p, w3=w3, wcols=wcols):
                    for k in range(16):
                        i = e.matmul(p[:, 0:wcols], lhsT=xnT[:, k, :], rhs=w3[:, k, 0:wcols], start=(k == 0), stop=(k == 15))
                    return i
                A("pe", f, [wtok, "xnT"], [ptok])
                if t in (4, 5):
                    A("act", lambda e, p=p, t=t: e.copy(out=v[:, (t - 4) * 512:(t - 3) * 512], in_=p[:, :]), [ptok], ["v"])
                elif t in (6, 7):
                    A("act", lambda e, p=p, t=t: e.activation(out=gs[:, (t - 6) * 512:(t - 5) * 512], in_=p[:, :], func=AF.Silu), [ptok], ["gs"])
                elif t in (8, 9):
                    A("act", lambda e, p=p, t=t: e.activation(out=zs[:, (t - 8) * 512:(t - 7) * 512], in_=p[:, :], func=AF.Silu), [ptok], ["zs"])
                else:
                    A("dve", lambda e, p=p: e.tensor_tensor(out=dtr[:, :], in0=p[:, 0:16], in1=dtb[:, :], op=ALU.add), [ptok, "dtb"], ["dtr"])
                    A("act", lambda e: e.activation(out=dtr[:, :], in_=dtr[:, :], func=AF.Exp), ["dtr"], ["dtr"])
                    A("act", lambda e: e.activation(out=dt[:, :], in_=dtr[:, :], func=AF.Ln, bias=1.0), ["dtr"], ["dt"])
        if mode == "halo":
            A("pool", lambda e: e.tensor_copy(out=xbc[:, :, 0:3], in_=xbc[:, :, 128:131]), ["xbc"], ["xbc"])
            A("pool", lambda e: e.tensor_copy(out=halo[:, :, :], in_=xbc[:, :, 0:3]), ["xbc"], ["halo"])
            continue
        if upto <= 2:
            break
        for blk in range(12):
            o = cvo[blk % 2]
            otok = f"cvo{blk % 2}"
            A("dve", lambda e, o=o, blk=blk: e.tensor_scalar(out=o[:, :], in0=xbc[:, blk, 0:128], scalar1=cw[:, blk, 0:1], scalar2=None, op0=ALU.mult), ["xbc", "cw"], [otok])
            for i in (1, 2, 3):
                A("dve", lambda e, o=o, blk=blk, i=i: e.scalar_tensor_tensor(out=o[:, :], in0=xbc[:, blk, i:i + 128], scalar=cw[:, blk, i:i + 1], in1=o[:, :], op0=ALU.mult, op1=ALU.add), ["xbc", "cw", otok], [otok])
            A("act", lambda e, o=o, blk=blk: e.activation(out=xc[:, blk, :], in_=o[:, :], func=AF.Silu, bias=cb[:, blk:blk + 1]), [otok, "cb"], ["xc"])
        A("pool", lambda e: e.tensor_copy(out=xbc[:, :, 0:3], in_=xbc[:, :, 128:131]), ["xbc"], ["xbc"])

        if upto <= 3:
            break
        DUMP("qT", qT[:, :, :].rearrange("p a b -> p (a b)"), "qT", 1024)
        DUMP("kT", kT[:, :, :].rearrange("p a b -> p (a b)"), "kT", 1024)
        DUMP("v", v[:, :], "v", 1024)
        DUMP("gs", gs[:, :], "gs", 1024)
        DUMP("xc", xc[:, :, :].rearrange("p a b -> p (a b)"), "xc", 1536)
        DUMP("dt", dt[:, :], "dt", 16)
        def f(e):
            for k in range(8):
                i = e.transpose(out=pt[0][:, k * 128:(k + 1) * 128], in_=kT[:, k, :], identity=identb[:, :])
            return i
        A("pe", f, ["kT", "identb"], ["pt0"])
        A("act", lambda e: e.copy(out=ktok[:, :], in_=pt[0][:, :]), ["pt0"], ["ktok"])
        for h in range(4):
            hs = slice(h * 256, (h + 1) * 256)
            if own:
                p, ptok = nextp()
                def f(e, p=p, h=h):
                    for hf in range(2):
                        i = e.matmul(p[:, 0:128], lhsT=kT[:, 2 * h + hf, :], rhs=qT[:, 2 * h + hf, :], start=(hf == 0), stop=(hf == 1))
                    return i
                A("pe", f, ["kT", "qT"], [ptok])
                A("dve", lambda e, p=p: e.tensor_tensor(out=scm[:, :], in0=p[:, 0:128], in1=causf[:, :], op=ALU.mult), [ptok, "causf"], ["scm"])
                py, pytok = nextp()
                def f(e, py=py, h=h, hs=hs):
                    e.matmul(py[:, 0:256], lhsT=scm[:, :], rhs=v[:, hs], start=True, stop=False)
                    for hf in range(2):
                        i = e.matmul(py[:, 0:256], lhsT=qT[:, 2 * h + hf, :], rhs=Sb[:, h, hf * 256:(hf + 1) * 256], start=False, stop=(hf == 1))
                    return i
                A("pe", f, ["scm", "v", "qT", "Sb"], [pytok])
                A("dve", lambda e, py=py: e.bn_stats(out=st6[:, :], in_=py[:, 0:256]), [pytok], ["st6"])
                A("dve", lambda e: e.bn_aggr(out=mv[:, :], in_=st6[:, :]), ["st6"], ["mv"])
                A("act", lambda e: e.activation(out=rs[:, :], in_=mv[:, 1:2], func=AF.Sqrt, bias=EPS), ["mv"], ["rs"])
                A("dve", lambda e: e.reciprocal(out=rs[:, :], in_=rs[:, :]), ["rs"], ["rs"])
                A("dve", lambda e, py=py: e.tensor_scalar(out=yn[:, :], in0=py[:, 0:256], scalar1=mv[:, 0:1], scalar2=rs[:, 0:1], op0=ALU.subtract, op1=ALU.mult), [pytok, "mv", "rs"], ["yn"])
                A("pool", lambda e, hs=hs: e.tensor_tensor(out=yn[:, :], in0=yn[:, :], in1=rnw[:, hs], op=ALU.mult), ["yn", "rnw"], ["yn"])
                A("pool", lambda e, hs=hs: e.tensor_tensor(out=ycat[:, hs], in0=yn[:, :], in1=gs[:, hs], op=ALU.mult), ["yn", "gs"], ["ycat"])
            pk, pktok = nextp()
            def f(e, pk=pk, h=h, hs=hs):
                for hf in range(2):
                    i = e.matmul(pk[:, hf * 256:(hf + 1) * 256], lhsT=ktok[:, h * 256 + hf * 128:h * 256 + (hf + 1) * 128], rhs=v[:, hs], start=True, stop=True)
                return i
            A("pe", f, ["ktok", "v"], [pktok])
            A("dve", lambda e, pk=pk, h=h: e.tensor_tensor(out=S[:, h, :], in0=pk[:, :], in1=S[:, h, :], op=ALU.add), [pktok, "S"], ["S"])
            A("pool", lambda e, h=h: e.tensor_scalar(out=S[:, h, :], in0=S[:, h, :], scalar1=Gb[:, h:h + 1], scalar2=None, op0=ALU.mult), ["S", "Gb"], ["S"])
            A("pool", lambda e, h=h: e.tensor_copy(out=Sb[:, h, :], in_=S[:, h, :]), ["S"], ["Sb"])

        if upto <= 4:
            break
        A("dve", lambda e: e.tensor_copy(out=dtv[:, :], in_=dt[:, :]), ["dt"], ["dtv"])
        A("dve", lambda e: e.tensor_tensor(out=aa[:, :], in0=dt[:, :], in1=negA[:, :], op=ALU.mult), ["dt", "negA"], ["aa"])
        pm, pmtok = nextp()
        def f(e, pm=pm):
            e.matmul(pm[:, 0:16], lhsT=U[:, :], rhs=aa[:, :], start=True, stop=True)
            return e.matmul(pm[:, 16:32], lhsT=ones[:, :], rhs=aa[:, :], start=True, stop=True)
        A("pe", f, ["U", "ones", "aa"], [pmtok])
        A("act", lambda e, pm=pm: e.copy(out=acs[:, :], in_=pm[:, 0:16]), [pmtok], ["acs"])
        if not own:
            A("dve", lambda e, pm=pm: e.tensor_tensor(out=Atot[:, :], in0=Atot[:, :], in1=pm[:, 16:32], op=ALU.add), [pmtok, "Atot"], ["Atot"])
        A("dve", lambda e, pm=pm: e.tensor_scalar(out=nacs[:, :], in0=pm[:, 0:16], scalar1=-1.0, scalar2=None, op0=ALU.mult), [pmtok], ["nacs"])
        A("act", lambda e, pm=pm: e.activation(out=cdec[:, :], in_=pm[:, 16:32], func=AF.Exp), [pmtok], ["cdec"])
        A("dve", lambda e, pm=pm: e.tensor_tensor(out=tmp16[:, :], in0=pm[:, 16:32], in1=acs[:, :], op=ALU.subtract), [pmtok, "acs"], ["tmp16"])
        A("act", lambda e: e.activation(out=te[:, :], in_=tmp16[:, :], func=AF.Exp), ["tmp16"], ["te"])
        A("act", lambda e: e.activation(out=el[:, :], in_=acs[:, :], func=AF.Exp), ["acs"], ["el"])
        def f(e):
            for k in range(8):
                i = e.transpose(out=pt[1][:, k * 128:(k + 1) * 128], in_=xc[:, k, :], identity=identb[:, :])
            return i
        A("pe", f, ["xc", "identb"], ["pt1"])
        A("act", lambda e: e.copy(out=xstok[:, :], in_=pt[1][:, :]), ["pt1"], ["xstok"])
        def f(e):
            for g in range(2):
                i = e.transpose(out=pt[0][:, g * 128:(g + 1) * 128], in_=xc[:, 8 + g, :], identity=identb[:, :])
            return i
        A("pe", f, ["xc", "identb"], ["pt0"])
        A("dve", lambda e: e.tensor_copy(out=Btok[:, :, :], in_=pt[0][:, 0:256].rearrange("p (g n) -> p g n", g=2)), ["pt0"], ["Btok"])
        A("dve", lambda e: e.tensor_tensor(out=v3(xdt[:, :], 16), in0=v3(xstok[:, :], 16), in1=bc(dtv[:, :].unsqueeze(2), [128, 16, 64]), op=ALU.mult), ["xstok", "dtv"], ["xdt"])
        A("dve", lambda e: e.tensor_tensor(out=v3(xdtw[:, :], 16), in0=v3(xdt[:, :], 16), in1=bc(te[:, :].unsqueeze(2), [128, 16, 64]), op=ALU.mult), ["xdt", "te"], ["xdtw"])
        if own:
            for g in range(2):
                p, ptok = nextp()
                A("pe", lambda e, p=p, g=g: e.matmul(p[:, 0:128], lhsT=xc[:, 8 + g, :], rhs=xc[:, 10 + g, :], start=True, stop=True), ["xc"], [ptok])
                A("act", lambda e, p=p, g=g: e.copy(out=cbt[:, g, :], in_=p[:, 0:128]), [ptok], ["cbt"])
            pyd = [(pg[4], "pg4"), (pg[5], "pg5")]
            for h in range(16):
                g = h // 8
                A("dve", lambda e, h=h: e.tensor_copy(out=abc[:, :], in_=bc(aa[:, h:h + 1], [128, 128])), ["aa"], ["abc"])
                p, ptok = nextp()
                def f(e, p=p):
                    e.matmul(p[:, 0:128], lhsT=abc[:, :], rhs=U[:, :], start=True, stop=False)
                    return e.matmul(p[:, 0:128], lhsT=identf[:, :], rhs=negm[:, :], start=False, stop=True)
                A("pe", f, ["abc", "U", "identf", "negm"], [ptok])
                A("act", lambda e, p=p, h=h: e.activation(out=dec[:, :], in_=p[:, 0:128], func=AF.Exp, bias=nacs[:, h:h + 1]), [ptok, "nacs"], ["dec"])
                A("pool", lambda e, g=g: e.tensor_tensor(out=Mb[:, :], in0=dec[:, :], in1=cbt[:, g, :], op=ALU.mult), ["dec", "cbt"], ["Mb"])
                A("pe", lambda e, h=h, g=g: e.matmul(pyd[g][0][:, (h % 8) * 64:(h % 8 + 1) * 64], lhsT=Mb[:, :], rhs=xdt[:, h * 64:(h + 1) * 64], start=True, stop=True), ["Mb", "xdt"], [pyd[g][1]])
            for g in range(2):
                gsl = slice(g * 512, (g + 1) * 512)
                po, potok = nextp()
                A("pe", lambda e, po=po, g=g, gsl=gsl: e.matmul(po[:, :], lhsT=xc[:, 10 + g, :], rhs=Hb[:, gsl], start=True, stop=True), ["xc", "Hb"], [potok])
                A("dve", lambda e, po=po, g=g, gsl=gsl: e.tensor_tensor(out=v3(ty[:, gsl], 8), in0=v3(po[:, :], 8), in1=bc(el[:, g * 8:(g + 1) * 8].unsqueeze(2), [128, 8, 64]), op=ALU.mult), [potok, "el"], ["ty"])
                A("dve", lambda e, g=g, gsl=gsl: e.tensor_tensor(out=ty[:, gsl], in0=ty[:, gsl], in1=pyd[g][0][:, :], op=ALU.add), ["ty", pyd[g][1]], ["ty"])
                A("pool", lambda e, g=g, gsl=gsl: e.tensor_tensor(out=v3(tu[:, :], 8), in0=v3(xstok[:, gsl], 8), in1=bc(dsk[:, g * 8:(g + 1) * 8].unsqueeze(2), [128, 8, 64]), op=ALU.mult), ["xstok", "dsk"], ["tu"])
                A("pool", lambda e, gsl=gsl: e.tensor_tensor(out=ty[:, gsl], in0=ty[:, gsl], in1=tu[:, :], op=ALU.add), ["ty", "tu"], ["ty"])
                A("pool", lambda e, gsl=gsl: e.tensor_tensor(out=ty[:, gsl], in0=ty[:, gsl], in1=zs[:, gsl], op=ALU.mult), ["ty", "zs"], ["ty"])
                A("act", lambda e, gsl=gsl: e.activation(out=tu[:, :], in_=ty[:, gsl], func=AF.Square, accum_out=ss2[:, 0:1]), ["ty"], ["ss2", "tu"])
                A("act", lambda e: e.activation(out=r2[:, :], in_=ss2[:, :], func=AF.Sqrt, scale=1.0 / 512, bias=EPS), ["ss2"], ["r2"])
                A("dve", lambda e: e.reciprocal(out=r2[:, :], in_=r2[:, :]), ["r2"], ["r2"])
                A("dve", lambda e, g=g, gsl=gsl: e.scalar_tensor_tensor(out=ycat[:, 1024 + g * 512:1024 + (g + 1) * 512], in0=ty[:, gsl], scalar=r2[:, 0:1], in1=snw[:, gsl], op0=ALU.mult, op1=ALU.mult), ["ty", "r2", "snw"], ["ycat"])
        for g in range(2):
            gsl = slice(g * 512, (g + 1) * 512)
            pn, pntok = nextp()
            A("pe", lambda e, pn=pn, g=g, gsl=gsl: e.matmul(pn[:, :], lhsT=Btok[:, g, :], rhs=xdtw[:, gsl], start=True, stop=True), ["Btok", "xdtw"], [pntok])
            A("dve", lambda e, g=g, gsl=gsl: e.tensor_tensor(out=v3(H[:, gsl], 8), in0=v3(H[:, gsl], 8), in1=bc(cdec[:, g * 8:(g + 1) * 8].unsqueeze(2), [128, 8, 64]), op=ALU.mult), ["H", "cdec"], ["H"])
            A("dve", lambda e, pn=pn, gsl=gsl: e.tensor_tensor(out=H[:, gsl], in0=H[:, gsl], in1=pn[:, :], op=ALU.add), ["H", pntok], ["H"])
            A("pool", lambda e, gsl=gsl: e.tensor_copy(out=Hb[:, gsl], in_=H[:, gsl]), ["H"], ["Hb"])
        if upto <= 5:
            break
        if not own:
            continue

        DUMP("ycat", ycat[:, :], "ycat", 2048)
        oc = jv
        A("dve", lambda e: e.tensor_copy(out=ycb[:, :], in_=ycat[:, :]), ["ycat"], ["xn"])
        transposes16(ycb, "xn", lambda half, oc=oc: yTall[:, half * 8:(half + 1) * 8, oc * 128:(oc + 1) * 128], "yTall")

    if upto > 5:
        own_list = own_ids
        A("dve", lambda e: e.memset(bars["dve"][:, 0:1], 0.0), [], list(s.tk.keys()) + ["bar_all"])
        allbars = ["bar_all"]
        stackM.close()
        stackE = contextlib.ExitStack()
        SBE = lambda n, sh, dt=F32: stackE.enter_context(nc.sbuf_tensor("sbe_" + n, sh, dt))
        gE = SBE("gE", [128, D]); hE = SBE("hE", [128, NOWN, D]); hnE = SBE("hnE", [128, D])
        xnE = SBE("xnE", [128, D], BF16); lobE = SBE("lobE", [128, D], BF16); loTE = SBE("loTE", [128, 16, 128], BF16)
        comb = SBE("comb", [128, NOWN, 32]); sgl = SBE("sgl", [128, 512]); actb = SBE("actb", [128, 512], BF16)
        actT = SBE("actT", [128, NOWN, 4, 128], BF16)
        hnTall = yTall
        def BAR(eng, fn, reads, writes, **kw):
            return A(eng, fn, list(reads) + allbars, writes, **kw)
        BAR("sp", lambda e: e.dma_start(out=gE[:, :], in_=g2_d[0:1, :].partition_broadcast(128)), [], ["gE"], dma="gE")
        for j in own_list:
            BAR("sp", lambda e, j=j: e.dma_start(out=hE[:, j, :], in_=x_d[(j + 1) * 128:(j + 2) * 128, :]), [], [f"hE{j}"], dma="xE")
        s.group_close("xE")
        for nb in range(4):
            nsl = slice(nb * 512, (nb + 1) * 512)
            src_tok[0] = "wb_out3"
            wb, wtok = wload(wb_out.ap()[nb].rearrange("(k p) n -> p k n", p=128), lambda b: b[:, :].rearrange("p (k n) -> p k n", k=16))
            w3 = wb[:, :].rearrange("p (k n) -> p k n", k=16)
            for j in own_list:
                p, ptok = nextp()
                def f(e, p=p, w3=w3, j=j):
                    for k in range(16):
                        i = e.matmul(p[:, :], lhsT=yTall[:, k, j * 128:(j + 1) * 128], rhs=w3[:, k, :], start=(k == 0), stop=(k == 15))
                    return i
                BAR("pe", f, [wtok, "yTall"], [ptok])
                BAR("dve", lambda e, p=p, nsl=nsl, j=j: e.tensor_tensor(out=hE[:, j, nsl], in0=hE[:, j, nsl], in1=p[:, :], op=ALU.add), [f"hE{j}", ptok], [f"hE{j}"])
        X = mybir.AxisListType.X
        for j in own_list:
            if dump == "h1" and j == own_list[0]:
                A("pool", lambda e, j=j: e.dma_start(out=dbg_d[:, :], in_=hE[:, j, :]), [f"hE{j}"], ["dbgd"], dma="dbg")
            jt = slice(j * 128, (j + 1) * 128)
            rms(hE[:, j, :], f"hE{j}", gE, "gE", hnE[:, :], "hnE", lobE[:, :], "lobE")
            BAR("dve", lambda e: e.tensor_copy(out=xnE[:, :], in_=hnE[:, :]), ["hnE"], ["xnE"])
            BAR("dve", lambda e: e.tensor_tensor(out=lobE[:, :], in0=hnE[:, :], in1=xnE[:, :], op=ALU.subtract), ["hnE", "xnE"], ["lobE"])
            transposes16(xnE, "xnE", lambda half, jt=jt: hnTall[:, half * 8:(half + 1) * 8, jt], "yTall")
            transposes16(lobE, "lobE", lambda half: loTE[:, half * 8:(half + 1) * 8, :], "loTE")
            p, ptok = nextp()
            def f(e, p=p, jt=jt):
                n = 0
                for (aT, w, sl) in ((hnTall, wrhi, jt), (loTE, wrhi, slice(0, 128)), (hnTall, wrlo, jt)):
                    for k in range(16):
                        i = e.matmul(p[:, 0:36], lhsT=aT[:, k, sl], rhs=w[:, k, :], start=(n == 0), stop=(n == 47))
                        n += 1
                return i
            A("pe", f, ["yTall", "loTE", "wrhi", "wrlo"], [ptok])
            A("dve", lambda e, p=p: e.tensor_tensor(out=lg[:, :], in0=p[:, 0:36], in1=rb[:, :], op=ALU.add), [ptok, "rb"], ["lg"])
            A("dve", lambda e: e.reduce_max(out=mx[:, :], in_=lg[:, 0:4], axis=X), ["lg"], ["mx"])
            A("dve", lambda e: e.tensor_scalar(out=ohg[:, :], in0=lg[:, 0:4], scalar1=mx[:, 0:1], scalar2=None, op0=ALU.is_equal), ["lg", "mx"], ["ohg"])
            A("dve", lambda e: e.tensor_scalar(out=nmx[:, :], in0=mx[:, :], scalar1=-1.0, scalar2=None, op0=ALU.mult), ["mx"], ["nmx"])
            A("act", lambda e: e.activation(out=eg[:, :], in_=lg[:, 0:4], func=AF.Exp, bias=nmx[:, 0:1], accum_out=sg[:, 0:1]), ["lg", "nmx"], ["eg", "sg"])
            A("dve", lambda e: e.reciprocal(out=psel[:, :], in_=sg[:, :]), ["sg"], ["psel"])
            A("dve", lambda e: e.tensor_scalar(out=les[:, :], in0=lg[:, 4:12], scalar1=ohg[:, 0:1], scalar2=None, op0=ALU.mult), ["lg", "ohg"], ["les"])
            for g in (1, 2, 3):
                A("dve", lambda e, g=g: e.scalar_tensor_tensor(out=les[:, :], in0=lg[:, 4 + 8 * g:12 + 8 * g], scalar=ohg[:, g:g + 1], in1=les[:, :], op0=ALU.mult, op1=ALU.add), ["lg", "ohg", "les"], ["les"])
            A("dve", lambda e: e.reduce_max(out=m1[:, :], in_=les[:, :], axis=X), ["les"], ["m1"])
            A("dve", lambda e: e.tensor_scalar(out=k1[:, :], in0=les[:, :], scalar1=m1[:, 0:1], scalar2=None, op0=ALU.is_equal), ["les", "m1"], ["k1"])
            A("dve", lambda e: e.scalar_tensor_tensor(out=les2[:, :], in0=k1[:, :], scalar=-1e30, in1=les[:, :], op0=ALU.mult, op1=ALU.add), ["k1", "les"], ["les2"])
            A("dve", lambda e: e.reduce_max(out=m2[:, :], in_=les2[:, :], axis=X), ["les2"], ["m2"])
            A("dve", lambda e: e.tensor_scalar(out=k2[:, :], in0=les2[:, :], scalar1=m2[:, 0:1], scalar2=None, op0=ALU.is_equal), ["les2", "m2"], ["k2"])
            A("dve", lambda e: e.tensor_tensor(out=dd[:, :], in0=m2[:, :], in1=m1[:, :], op=ALU.subtract), ["m1", "m2"], ["dd"])
            A("act", lambda e: e.activation(out=ed[:, :], in_=dd[:, :], func=AF.Exp), ["dd"], ["ed"])
            A("dve", lambda e: e.tensor_scalar(out=w1[:, :], in0=ed[:, :], scalar1=1.0, scalar2=None, op0=ALU.add), ["ed"], ["w1"])
            A("dve", lambda e: e.reciprocal(out=w1[:, :], in_=w1[:, :]), ["w1"], ["w1"])
            A("dve", lambda e: e.tensor_tensor(out=w2[:, :], in0=ed[:, :], in1=w1[:, :], op=ALU.mult), ["ed", "w1"], ["w2"])
            A("dve", lambda e: e.tensor_scalar(out=wig[:, :], in0=k1[:, :], scalar1=w1[:, 0:1], scalar2=None, op0=ALU.mult), ["k1", "w1"], ["wig"])
            A("dve", lambda e: e.scalar_tensor_tensor(out=wig[:, :], in0=k2[:, :], scalar=w2[:, 0:1], in1=wig[:, :], op0=ALU.mult, op1=ALU.add), ["k2", "w2", "wig"], ["wig"])
            for g in range(4):
                A("dve", lambda e, g=g: e.tensor_tensor(out=scg[:, :], in0=ohg[:, g:g + 1], in1=psel[:, :], op=ALU.mult), ["ohg", "psel"], ["scg"])
                BAR("dve", lambda e, g=g, j=j: e.tensor_scalar(out=comb[:, j, g * 8:(g + 1) * 8], in0=wig[:, :], scalar1=scg[:, 0:1], scalar2=None, op0=ALU.mult), ["wig", "scg"], ["comb"])
        if upto > 7:
            kview = lambda b: b[:, :].rearrange("p (k n) -> p k n", k=16)
            for ex in range(32):
                src_tok[0] = "wb_d31"
                wgb, wgtok = wload(wb_g.ap()[ex].rearrange("(k p) n -> p k n", p=128), kview)
                wub, wutok = wload(wb_u.ap()[ex].rearrange("(k p) n -> p k n", p=128), kview)
                for j in own_list:
                    jt = slice(j * 128, (j + 1) * 128)
                    pgt, pgtok = nextp()
                    put, putok = nextp()
                    def f(e, pp=pgt, w3=kview(wgb), jt=jt):
                        for k in range(16):
                            i = e.matmul(pp[:, :], lhsT=hnTall[:, k, jt], rhs=w3[:, k, :], start=(k == 0), stop=(k == 15))
                        return i
                    A("pe", f, [wgtok, "yTall"], [pgtok])
                    def f(e, pp=put, w3=kview(wub), jt=jt):
                        for k in range(16):
                            i = e.matmul(pp[:, :], lhsT=hnTall[:, k, jt], rhs=w3[:, k, :], start=(k == 0), stop=(k == 15))
                        return i
                    A("pe", f, [wutok, "yTall"], [putok])
                    BAR("act", lambda e, pp=pgt: e.activation(out=sgl[:, :], in_=pp[:, :], func=AF.Silu), [pgtok], ["sgl"])
                    BAR("dve", lambda e, pp=put, ex=ex, j=j: e.scalar_tensor_tensor(out=actb[:, :], in0=sgl[:, :], scalar=comb[:, j, ex:ex + 1], in1=pp[:, :], op0=ALU.mult, op1=ALU.mult), ["sgl", "comb", putok], ["actb"])
                    def f(e):
                        for k in range(4):
                            i = e.transpose(out=pt[0][:, k * 128:(k + 1) * 128], in_=actb[:, k * 128:(k + 1) * 128], identity=identb[:, :])
                        return i
                    A("pe", f, ["actb", "identb"], ["pt0"])
                    BAR("act", lambda e, j=j: e.copy(out=actT[:, j, :, :], in_=pt[0][:, 0:512].rearrange("p (k t) -> p k t", k=4)), ["pt0"], [f"actT{j}"])
                wdb, wdtok = wload(wb_d.ap()[ex].rearrange("(k p) n -> p k n", p=128), lambda b: b[:, :].rearrange("p (k n) -> p k n", k=4))
                wd3 = wdb[:, :].rearrange("p (k n) -> p k n", k=4)
                for j in own_list:
                    for nb in range(4):
                        nsl = slice(nb * 512, (nb + 1) * 512)
                        pd, pdtok = nextp()
                        def f(e, pd=pd, wd3=wd3, nsl=nsl, j=j):
                            for k in range(4):
                                i = e.matmul(pd[:, :], lhsT=actT[:, j, k, :], rhs=wd3[:, k, nsl], start=(k == 0), stop=(k == 3))
                            return i
                        A("pe", f, [wdtok, f"actT{j}"], [pdtok])
                        A("dve", lambda e, pd=pd, nsl=nsl, j=j: e.tensor_tensor(out=hE[:, j, nsl], in0=hE[:, j, nsl], in1=pd[:, :], op=ALU.add), [f"hE{j}", pdtok], [f"hE{j}"])
            A("sp", lambda e: e.dma_start(out=gE[:, :], in_=gf_d[0:1, :].partition_broadcast(128)), [], ["gE"], dma="gE")
            for j in own_list:
                rms(hE[:, j, :], f"hE{j}", gE, "gE", hnE[:, :], "hnE", lobE[:, :], "lobE")
                A("sp", lambda e, j=j: e.dma_start(out=out_d[j * 128:(j + 1) * 128, :], in_=hnE[:, :]), ["hnE"], ["outd"], dma="out")
        s.emit()
        stackE.close()
        stackP.close()
        return nc

    s.emit()
    stackM.close()
    stackP.close()
    return nc


_NC_CACHE = {}


def kernel(x, positions, norm1_w, w_in, conv_w, conv_b, dt_bias, a_log, d_skip, ret_norm_w,
           ssm_norm_w, w_out, norm2_w, w_router_group, b_router_group, w_router_expert,
           b_router_expert, w_expert_gate, w_expert_up, w_expert_down, final_norm_w):
    f32 = np.float32
    x2 = np.asarray(x, f32).reshape(SEQ, D)
    pos = np.asarray(positions, np.int32).reshape(1, SEQ)
    idx = np.arange(128, dtype=np.float64)
    gam = 1.0 - 2.0 ** (-5.0 - np.arange(4, dtype=np.float64))
    rqv = (gam[:, None] ** (idx[None, :] + 1)).astype(f32).reshape(1, 512)
    rkv = ((gam[:, None] ** (-(idx[None, :] + 1))) * (256 ** -0.5)).astype(f32).reshape(1, 512)
    Gv = (gam ** 128).astype(f32).reshape(1, 4)
    invf = (10000.0 ** (-np.arange(128, dtype=f32) / 128)).astype(f32).reshape(128, 1)
    ident = np.eye(128, dtype=f32)
    si, li = np.meshgrid(np.arange(128), np.arange(128), indexing="ij")
    caus = (li >= si).astype(f32)
    Um = (si <= li).astype(f32)
    negm = np.where(li >= si, 0.0, -30000.0).astype(f32)
    shared = {
        "norm1_w": np.asarray(norm1_w, f32).reshape(1, D),
        "w_in": np.asarray(w_in, f32).reshape(D, INP),
        "conv_w": np.ascontiguousarray(np.asarray(conv_w, f32).reshape(4, 12, 128).transpose(2, 1, 0)),
        "conv_b": np.ascontiguousarray(np.asarray(conv_b, f32).reshape(12, 128).T),
        "dt_bias": np.asarray(dt_bias, f32).reshape(1, 16),
        "a_log": np.asarray(a_log, f32).reshape(1, 16),
        "d_skip": np.asarray(d_skip, f32).reshape(1, 16),
        "ret_norm_w": np.asarray(ret_norm_w, f32).reshape(1, 1024),
        "ssm_norm_w": np.asarray(ssm_norm_w, f32).reshape(1, 1024),
        "w_out": np.asarray(w_out, f32).reshape(D, D),
        "norm2_w": np.asarray(norm2_w, f32).reshape(1, D),
        "w_router": np.ascontiguousarray(np.concatenate([np.asarray(w_router_group, f32).reshape(D, 4), np.asarray(w_router_expert, f32).reshape(D, 32)], axis=1)),
        "b_router": np.concatenate([np.asarray(b_router_group, f32).reshape(1, 4), np.asarray(b_router_expert, f32).reshape(1, 32)], axis=1),
        "w_gate": np.asarray(w_expert_gate, f32).reshape(32, D, 512),
        "w_up": np.asarray(w_expert_up, f32).reshape(32, D, 512),
        "w_down": np.asarray(w_expert_down, f32).reshape(32, 512, D),
        "final_norm_w": np.asarray(final_norm_w, f32).reshape(1, D),
        "ident": ident, "caus": caus, "U": Um, "negm": negm, "rq": rqv, "rk": rkv, "G": Gv, "inv": invf,
    }
    in_maps = []
    gam64 = 1.0 - 2.0 ** (-5.0 - np.arange(4, dtype=np.float64))
    for i in range(NCORE):
        xe = np.zeros((TOK + 128, D), f32)
        pe_ = np.zeros((1, TOK + 128), np.int32)
        if i > 0:
            xe[:128] = x2[TOK * i - 128:TOK * i]
            pe_[0, :128] = pos[0, TOK * i - 128:TOK * i]
        xe[128:] = x2[TOK * i:TOK * (i + 1)]
        pe_[0, 128:] = pos[0, TOK * i:TOK * (i + 1)]
        cRv = np.zeros((8, 4), np.float64)
        Miv = np.zeros((8, 8), f32)
        viv = np.zeros((1, 8), f32)
        for m in range(i):
            cRv[m] = (gam64 ** 128) ** (8 * (i - 1 - m))
            viv[0, m] = 1.0
            for m2 in range(m + 1, i):
                Miv[m, m2] = 1.0
        mm = dict(shared)
        mm["x"] = xe
        mm["pos"] = pe_
        mm["cR"] = cRv.astype(f32).reshape(1, 32)
        mm["Mi"] = Miv.reshape(1, 64)
        mm["vi"] = viv
        in_maps.append(mm)
    if "nc" not in _NC_CACHE:
        _NC_CACHE["nc"] = build_nc()
    res = run_bass_kernel_spmd(_NC_CACHE["nc"], in_maps, core_ids=list(range(NCORE)))
    out = np.concatenate([np.asarray(r["out"], f32) for r in res.results], axis=0)
    return out.reshape(1, SEQ, D)
```
